# Optimizing a Trainium2 kernel written in Bass

```python
import math
import jax, jax.numpy as jnp
from jax import lax
import numpy as np

D_MODEL = 1024
BATCH = 8
SEQ = 4096
DEPTH = 1

EPS = 1e-6
ROPE_THETA = 500000.0
ROPE_FRACTION = 4
ROT_HEAD_DIM = 64
ROPE_DIM = ROT_HEAD_DIM // ROPE_FRACTION
Q_BLOCK = 128
MAX_POS_OFFSET = 2048

DA_HEADS = 4
DA_QK_DIM = 64
DA_V_DIM = 2 * DA_QK_DIM
DA_WIDTH = DA_HEADS * DA_V_DIM
DS_HEADS = 8
DS_KV_HEADS = 2
DS_HEAD_DIM = 64
DS_WIDTH = DS_HEADS * DS_HEAD_DIM
IDX_HEADS = 8
IDX_DIM = 64
TOPK_MAX = 256
MEM_TOKENS = 256
MEM_HEADS = 4
MEM_HEAD_DIM = 128
MEM_WIDTH = MEM_HEADS * MEM_HEAD_DIM
PEER_HEADS = 8
PEER_N_KEYS = 128
PEER_N_EXPERTS = PEER_N_KEYS * PEER_N_KEYS
PEER_QUERY_DIM = 128
PEER_HALF = PEER_QUERY_DIM // 2
PEER_TOPK = 16
PEER_CHUNK = 128

DA_Q_COLS = DA_HEADS * 2 * DA_QK_DIM
DA_K_COLS = DA_HEADS * 2 * DA_QK_DIM
DA_V_COLS = DA_WIDTH
DS_Q_COLS = DS_WIDTH
DS_K_COLS = DS_KV_HEADS * DS_HEAD_DIM
DS_V_COLS = DS_KV_HEADS * DS_HEAD_DIM
IX_Q_COLS = IDX_HEADS * IDX_DIM
IX_K_COLS = IDX_DIM
IX_W_COLS = IDX_HEADS
GATE_COLS = 2 * D_MODEL
COL_SIZES = (DA_Q_COLS, DA_K_COLS, DA_V_COLS, DS_Q_COLS, DS_K_COLS, DS_V_COLS, IX_Q_COLS, IX_K_COLS, IX_W_COLS, GATE_COLS)
SPLITS = tuple(int(c) for c in np.cumsum(COL_SIZES)[:-1])
IN_COLS = int(sum(COL_SIZES))

kernel_name = "hybrid_diffattn_dsa_peer_block"


def rms_norm(x, g):
    xf = x.astype(jnp.float32)
    y = xf * lax.rsqrt(jnp.mean(xf * xf, axis=-1, keepdims=True) + EPS)
    return (y * g.astype(jnp.float32)).astype(x.dtype)


def rope_tables(positions):
    half = ROPE_DIM // 2
    inv_freq = ROPE_THETA ** (-(jnp.arange(half, dtype=jnp.float32) * 2.0) / ROPE_DIM)
    ang = positions.astype(jnp.float32)[..., None] * inv_freq
    return jnp.cos(ang), jnp.sin(ang)


def apply_partial_rope(t, cos, sin):
    r = t.shape[-1] // ROPE_FRACTION
    half = r // 2
    bshape = cos.shape[:2] + (1,) * (t.ndim - 3) + (half,)
    c = cos.reshape(bshape).astype(t.dtype)
    s = sin.reshape(bshape).astype(t.dtype)
    t1 = t[..., :half]
    t2 = t[..., half:r]
    return jnp.concatenate([t1 * c - t2 * s, t2 * c + t1 * s, t[..., r:]], axis=-1)


def to_blocks(a):
    b, s = a.shape[:2]
    return jnp.moveaxis(a.reshape((b, s // Q_BLOCK, Q_BLOCK) + a.shape[2:]), 1, 0)


def from_blocks(a):
    a = jnp.moveaxis(a, 0, 1)
    return a.reshape((a.shape[0], a.shape[1] * a.shape[2]) + a.shape[3:])


def diff_attention(q, k, v, lam, lam_init, subln_g):
    b, s_len = q.shape[:2]
    scale = DA_QK_DIM ** -0.5
    key_pos = jnp.arange(s_len)

    def block(args):
        qb, i = args
        sc = jnp.einsum('bqhmd,bshmd->bhmqs', qb, k).astype(jnp.float32) * scale
        qpos = i * Q_BLOCK + jnp.arange(Q_BLOCK)
        causal = key_pos[None, :] <= qpos[:, None]
        sc = jnp.where(causal, sc, -jnp.inf)
        p = jax.nn.softmax(sc, axis=-1)
        a = p[:, :, 0] - lam * p[:, :, 1]
        return jnp.einsum('bhqs,bshd->bqhd', a.astype(v.dtype), v)

    o = from_blocks(lax.map(block, (to_blocks(q), jnp.arange(s_len // Q_BLOCK))))
    o = rms_norm(o, subln_g) * (1.0 - lam_init)
    return o.reshape(b, s_len, DA_WIDTH)


def dsa_attention(q, k, v, qi, ki, wi):
    b, s_len = q.shape[:2]
    n_sel = min(TOPK_MAX, s_len // 4)
    key_pos = jnp.arange(s_len)
    scale = DS_HEAD_DIM ** -0.5
    take = jax.vmap(lambda a, idx: a[idx])

    def block(args):
        qb, qib, wib, i = args
        qpos = i * Q_BLOCK + jnp.arange(Q_BLOCK)
        causal = key_pos[None, :] <= qpos[:, None]
        idx_s = jax.nn.relu(jnp.einsum('bqhd,bsd->bqhs', qib, ki))
        idx_s = jnp.einsum('bqh,bqhs->bqs', wib, idx_s).astype(jnp.float32)
        idx_s = jnp.where(causal[None], idx_s, -jnp.inf)
        _, sel = lax.top_k(idx_s, n_sel)
        valid = sel <= qpos[None, :, None]
        ks = take(k, sel)
        vs = take(v, sel)
        qg = qb.reshape(b, Q_BLOCK, DS_KV_HEADS, DS_HEADS // DS_KV_HEADS, DS_HEAD_DIM)
        sc = jnp.einsum('bqgrd,bqkgd->bqgrk', qg, ks).astype(jnp.float32) * scale
        sc = jnp.where(valid[:, :, None, None, :], sc, -jnp.inf)
        p = jax.nn.softmax(sc, axis=-1)
        o = jnp.einsum('bqgrk,bqkgd->bqgrd', p.astype(vs.dtype), vs)
        return o.reshape(b, Q_BLOCK, DS_WIDTH)

    o = lax.map(block, (to_blocks(q), to_blocks(qi), to_blocks(wi), jnp.arange(s_len // Q_BLOCK)))
    return from_blocks(o)


def memory_attention(h, mem_n, w_q, w_kv, w_o):
    b, s_len, _ = h.shape
    m = mem_n.shape[1]
    q = (h @ w_q).reshape(b, s_len, MEM_HEADS, MEM_HEAD_DIM)
    kv = (mem_n @ w_kv).reshape(b, m, 2, MEM_HEADS, MEM_HEAD_DIM)
    k, v = kv[:, :, 0], kv[:, :, 1]
    sc = jnp.einsum('bshd,bmhd->bhsm', q, k).astype(jnp.float32) * (MEM_HEAD_DIM ** -0.5)
    p = jax.nn.softmax(sc, axis=-1)
    o = jnp.einsum('bhsm,bmhd->bshd', p.astype(v.dtype), v).reshape(b, s_len, MEM_WIDTH)
    return o @ w_o


def peer_ffn(h, w_q, sub_keys, u, v):
    b, s_len, d = h.shape
    n = b * s_len
    hf = h.reshape(n, d)
    q = (hf @ w_q).reshape(n, PEER_HEADS, 2, PEER_HALF)
    sc = jnp.einsum('nhcd,hckd->nhck', q, sub_keys).astype(jnp.float32)
    s_top, i_top = lax.top_k(sc, PEER_TOPK)
    cand_s = s_top[:, :, 0, :, None] + s_top[:, :, 1, None, :]
    cand_i = i_top[:, :, 0, :, None] * PEER_N_KEYS + i_top[:, :, 1, None, :]
    cand_s = cand_s.reshape(n, PEER_HEADS, PEER_TOPK * PEER_TOPK)
    cand_i = cand_i.reshape(n, PEER_HEADS, PEER_TOPK * PEER_TOPK)
    best_s, pos = lax.top_k(cand_s, PEER_TOPK)
    best_i = jnp.take_along_axis(cand_i, pos, axis=-1)
    g = jax.nn.softmax(best_s, axis=-1)
    idx = best_i.reshape(n, PEER_HEADS * PEER_TOPK)
    g = g.reshape(n, PEER_HEADS * PEER_TOPK)
    nc = n // PEER_CHUNK

    def chunk(args):
        hc, ic, gc = args
        a = jnp.einsum('cd,ced->ce', hc, u[ic])
        a = jax.nn.gelu(a, approximate=False) * gc.astype(hc.dtype)
        return jnp.einsum('ce,ced->cd', a, v[ic])

    out = lax.map(chunk, (hf.reshape(nc, PEER_CHUNK, d), idx.reshape(nc, PEER_CHUNK, -1), g.reshape(nc, PEER_CHUNK, -1)))
    return out.reshape(b, s_len, d)


def setup_inputs(seed: int = 0) -> dict:
    key = jax.random.key(seed)
    ks = jax.random.split(key, 24)
    f32 = jnp.float32

    def nrm(k, shape, scale):
        return jax.random.normal(k, shape, f32) * scale

    def gain(k, shape):
        return 1.0 + 0.02 * jax.random.normal(k, shape, f32)

    D = D_MODEL
    x = nrm(ks[0], (BATCH, SEQ, D), 1.0)
    mem = nrm(ks[1], (BATCH, MEM_TOKENS, D), 1.0)
    positions = jnp.arange(SEQ, dtype=jnp.int32)[None, :] + jax.random.randint(ks[2], (BATCH, 1), 0, MAX_POS_OFFSET, dtype=jnp.int32)
    return {
        "x": x,
        "mem": mem,
        "positions": positions,
        "norm_mix_g": gain(ks[3], (DEPTH, D)),
        "w_in": nrm(ks[4], (DEPTH, D, IN_COLS), D ** -0.5),
        "da_lambda": nrm(ks[5], (DEPTH, 4, DA_QK_DIM), 0.1),
        "da_subln_g": gain(ks[6], (DEPTH, DA_V_DIM)),
        "w_branch_a": nrm(ks[7], (DEPTH, DA_WIDTH, D), DA_WIDTH ** -0.5),
        "w_branch_b": nrm(ks[8], (DEPTH, DS_WIDTH, D), DS_WIDTH ** -0.5),
        "gate_bias": nrm(ks[9], (DEPTH, GATE_COLS), 0.02),
        "w_out": nrm(ks[10], (DEPTH, D, D), D ** -0.5),
        "norm_mem_g": gain(ks[11], (DEPTH, D)),
        "mem_kv_norm_g": gain(ks[12], (DEPTH, D)),
        "w_mem_q": nrm(ks[13], (DEPTH, D, MEM_WIDTH), D ** -0.5),
        "w_mem_kv": nrm(ks[14], (DEPTH, D, 2 * MEM_WIDTH), D ** -0.5),
        "w_mem_o": nrm(ks[15], (DEPTH, MEM_WIDTH, D), MEM_WIDTH ** -0.5),
        "norm_ffn_g": gain(ks[16], (DEPTH, D)),
        "peer_w_q": nrm(ks[17], (DEPTH, D, PEER_HEADS * PEER_QUERY_DIM), D ** -0.5),
        "peer_sub_keys": nrm(ks[18], (DEPTH, PEER_HEADS, 2, PEER_N_KEYS, PEER_HALF), PEER_HALF ** -0.5),
        "peer_u": nrm(ks[19], (DEPTH, PEER_N_EXPERTS, D), D ** -0.5),
        "peer_v": nrm(ks[20], (DEPTH, PEER_N_EXPERTS, D), PEER_TOPK ** -0.5),
        "final_norm_g": gain(ks[21], (D,)),
    }


def reference(x, mem, positions, norm_mix_g, w_in, da_lambda, da_subln_g, w_branch_a, w_branch_b, gate_bias, w_out, norm_mem_g, mem_kv_norm_g, w_mem_q, w_mem_kv, w_mem_o, norm_ffn_g, peer_w_q, peer_sub_keys, peer_u, peer_v, final_norm_g):
    b, s_len, d = x.shape
    cos, sin = rope_tables(positions)
    for l in range(DEPTH):
        h = rms_norm(x, norm_mix_g[l])
        proj = h @ w_in[l]
        da_q, da_k, da_v, ds_q, ds_k, ds_v, ix_q, ix_k, ix_w, gates = jnp.split(proj, SPLITS, axis=-1)

        lam_init = 0.8 - 0.6 * math.exp(-0.3 * l)
        lp = da_lambda[l].astype(jnp.float32)
        lam = jnp.exp(jnp.sum(lp[0] * lp[1])) - jnp.exp(jnp.sum(lp[2] * lp[3])) + lam_init
        qa = apply_partial_rope(da_q.reshape(b, s_len, DA_HEADS, 2, DA_QK_DIM), cos, sin)
        ka = apply_partial_rope(da_k.reshape(b, s_len, DA_HEADS, 2, DA_QK_DIM), cos, sin)
        va = da_v.reshape(b, s_len, DA_HEADS, DA_V_DIM)
        o_a = diff_attention(qa, ka, va, lam, lam_init, da_subln_g[l])

        qb = apply_partial_rope(ds_q.reshape(b, s_len, DS_HEADS, DS_HEAD_DIM), cos, sin)
        kb = apply_partial_rope(ds_k.reshape(b, s_len, DS_KV_HEADS, DS_HEAD_DIM), cos, sin)
        vb = ds_v.reshape(b, s_len, DS_KV_HEADS, DS_HEAD_DIM)
        qi = apply_partial_rope(ix_q.reshape(b, s_len, IDX_HEADS, IDX_DIM), cos, sin)
        ki = apply_partial_rope(ix_k, cos, sin)
        o_b = dsa_attention(qb, kb, vb, qi, ki, ix_w)

        g = jax.nn.sigmoid((gates + gate_bias[l]).astype(jnp.float32)).astype(x.dtype)
        mix = g[..., :d] * (o_a @ w_branch_a[l]) + g[..., d:] * (o_b @ w_branch_b[l])
        x = x + mix @ w_out[l]

        x = x + memory_attention(rms_norm(x, norm_mem_g[l]), rms_norm(mem, mem_kv_norm_g[l]), w_mem_q[l], w_mem_kv[l], w_mem_o[l])

        x = x + peer_ffn(rms_norm(x, norm_ffn_g[l]), peer_w_q[l], peer_sub_keys[l], peer_u[l], peer_v[l])
    return rms_norm(x, final_norm_g)
```

```python
import math
import os
from contextlib import ExitStack

import numpy as np
import concourse.bass as bass
import concourse.mybir as mybir
from concourse.bass_utils import run_bass_kernel_spmd

F32 = mybir.dt.float32
BF16 = mybir.dt.bfloat16
I32 = mybir.dt.int32
U32 = mybir.dt.uint32
ALU = mybir.AluOpType
AF = mybir.ActivationFunctionType
AX = mybir.AxisListType

EPOCH = 30000
P = 128
SL = 4096
NT = 32
D = 1024
KC = 8
EPS = 1e-6
NEG = -1.0e30
NSEL = 256
NBIS = 16
PI = float(np.pi)


class Sched:
    def __init__(self, nc, same_engine_sync=True):
        self.nc = nc
        self.engs = {"pe": nc.tensor, "dve": nc.vector, "act": nc.scalar,
                     "pool": nc.gpsimd, "sp": nc.sync}
        self.sem = {}
        self.cnt = {}
        self.nsem = 0
        self.waited = {e: {} for e in self.engs}
        self.same = same_engine_sync
        self.res = {}
        self.dma_sem = {}
        self.dma_total = {}
        self.all_dma = []
        self.ninst = {e: 0 for e in self.engs}

    def _newsem(self, name):
        self.nsem += 1
        return self.nc.alloc_semaphore(f"{name}_{self.nsem}")

    def _eng_token(self, e):
        if e not in self.sem or self.cnt[e] >= EPOCH:
            self.sem[e] = self._newsem(f"s_{e}")
            self.cnt[e] = 0
        self.cnt[e] += 1
        return (self.sem[e], self.cnt[e], e)

    def _wait(self, e, tok):
        sem, val, src = tok
        if src == "dma":
            val = self.dma_total[sem.name]
        elif src == e and (e == "pe" or not self.same):
            return
        w = self.waited[e]
        if w.get(sem.name, 0) >= val:
            return
        self.engs[e].wait_ge(sem, val)
        w[sem.name] = val

    def _deps(self, e, reads, writes):
        for k in reads:
            r = self.res.get(k)
            if r is not None:
                for t in r[0]:
                    self._wait(e, t)
        for k in writes:
            r = self.res.get(k)
            if r is not None:
                for t in r[0]:
                    self._wait(e, t)
                for t in r[1]:
                    self._wait(e, t)

    def _record(self, tok, reads, writes):
        for k in reads:
            r = self.res.setdefault(k, [[], []])
            r[1].append(tok)
            if len(r[1]) > 48:
                r[1] = self._compact(r[1])
        for k in writes:
            self.res[k] = [[tok], []]

    @staticmethod
    def _compact(toks):
        best = {}
        for t in toks:
            key = (t[0].name, t[2])
            if key not in best or best[key][1] < t[1]:
                best[key] = t
        return list(best.values())

    def op(self, e, fn, reads=(), writes=()):
        psr = [k for k in reads if k.startswith("ps")]
        if psr:
            reads = [k for k in reads if not k.startswith("ps")]
            writes = list(writes) + [k for k in psr if k not in writes]
        self._deps(e, reads, writes)
        ins = fn(self.engs[e])
        tok = self._eng_token(e)
        ins.then_inc(tok[0], 1)
        self._record(tok, reads, writes)
        self.ninst[e] += 1
        return tok

    def dma(self, q, fn, semkey, reads=(), writes=()):
        self._deps(q, reads, writes)
        if semkey not in self.dma_sem or self.dma_total[self.dma_sem[semkey].name] + 16 > EPOCH:
            s = self._newsem("d")
            self.dma_sem[semkey] = s
            self.dma_total[s.name] = 0
            self.all_dma.append(s)
        s = self.dma_sem[semkey]
        ins = fn(self.engs[q])
        self.dma_total[s.name] += 16
        ins.then_inc(s, 16)
        tok = (s, self.dma_total[s.name], "dma")
        self._record(tok, reads, writes)
        self.ninst[q] += 1
        return tok

    def barrier(self):
        toks = []
        for e in ("pe", "dve", "act", "pool"):
            if e in self.sem:
                toks.append((self.sem[e], self.cnt[e], e))
        for e in self.engs:
            for t in toks:
                if t[2] != e:
                    self._wait(e, t)
            for s in self.all_dma:
                if self.dma_total[s.name] > 0:
                    self._wait(e, (s, self.dma_total[s.name], "dma"))
        self.res = {}


C_DAQ, C_DAK, C_DAV, C_DSQ, C_DSK, C_DSV, C_IXQ, C_IXK, C_IXW, C_GATE = (
    0, 512, 1024, 1536, 2048, 2176, 2304, 2816, 2880, 2888)


def build_program(debug=False, stop_after=None):
    nc = bass.Bass("TRN2", target_bir_lowering=False)
    S = Sched(nc, same_engine_sync=True)

    def din(name, shape, dt=F32):
        return nc.dram_tensor(name, shape, dt, kind="ExternalInput").ap()

    x_d = din("x", [SL, D])
    mem_d = din("mem", [256, D])
    pos_d = din("pos", [P, NT], I32)
    g_mix_d = din("norm_mix_g", [D])
    w_in_d = din("w_in", [D, 4936])
    lam_d = din("da_lambda", [256])
    subg_d = din("da_subln_g", [128])
    wa_d = din("w_branch_a", [512, D])
    wb_d = din("w_branch_b", [512, D])
    gbias_d = din("gate_bias", [2048])
    wo_d = din("w_out", [D, D])
    g_mem_d = din("norm_mem_g", [D])
    g_kv_d = din("mem_kv_norm_g", [D])
    wmq_d = din("w_mem_q", [D, 512])
    wmkv_d = din("w_mem_kv", [D, D])
    wmo_d = din("w_mem_o", [512, D])
    g_ffn_d = din("norm_ffn_g", [D])
    wpq_d = din("peer_w_q", [D, D])
    skt_d = din("peer_skt", [P, 8, 128])
    pu_d = din("peer_u", [16384, D])
    pv_d = din("peer_v", [16384, D])
    g_fin_d = din("final_norm_g", [D])
    out_d = nc.dram_tensor("out", [SL, D], F32, kind="ExternalOutput").ap()

    hnT_d = nc.dram_tensor("hnT_scr", [NT, P, KC * 128], BF16).ap()
    oaT_d = nc.dram_tensor("oaT_scr", [NT, P, 4 * 128], BF16).ap()
    obT_d = nc.dram_tensor("obT_scr", [NT, P, 4 * 128], BF16).ap()
    x2_d = nc.dram_tensor("x2_scr", [NT, P, D], F32).ap()
    puv_d = nc.dram_tensor("puv_scr", [16384, 2, D], BF16).ap()

    dbg = {}
    if debug:
        dbg["oa"] = nc.dram_tensor("dbg_oa", [NT, P, 512], BF16, kind="ExternalOutput").ap()
        dbg["ob"] = nc.dram_tensor("dbg_ob", [NT, P, 512], BF16, kind="ExternalOutput").ap()
        dbg["kt"] = nc.dram_tensor("dbg_kt", [P, 4 * SL], BF16, kind="ExternalOutput").ap()
        dbg["x2"] = nc.dram_tensor("dbg_x2", [NT, P, D], F32, kind="ExternalOutput").ap()
        dbg["x1"] = nc.dram_tensor("dbg_x1", [NT, P, D], F32, kind="ExternalOutput").ap()

    xT = x_d.rearrange("(t p) d -> t p d", p=P)
    outT = out_d.rearrange("(t p) d -> t p d", p=P)

    PS = nc.alloc_psum_tensor("PS", [P, 8, 512], F32)

    def psbf(b):
        return PS[:, b, :].bitcast(BF16)

    def pk(*bs):
        return ["ps%d" % b for b in bs]

    gstack = ExitStack()

    def GT(name, shape, dt):
        return gstack.enter_context(nc.sbuf_tensor(name, shape, dt))

    iot = GT("iot", [P, P], I32)
    ident = GT("ident", [P, P], BF16)
    tri01 = GT("tri01", [P, P], BF16)
    triNEG = GT("triNEG", [P, P], F32)
    CC = GT("CC", [P, NT, 16], F32)
    SSt = GT("SSt", [P, NT, 16], F32)
    neglam = GT("neglam", [P, 1], F32)
    subg08 = GT("subg08", [P, 128], F32)

    S.op("pool", lambda e: e.iota(iot[:], pattern=[[1, P]], base=0, channel_multiplier=-1), writes=["iot"])
    S.op("dve", lambda e: e.tensor_scalar(out=ident[:], in0=iot[:], scalar1=0.0, scalar2=None, op0=ALU.is_equal),
         reads=["iot"], writes=["ident"])
    S.op("dve", lambda e: e.tensor_scalar(out=tri01[:], in0=iot[:], scalar1=0.0, scalar2=None, op0=ALU.is_ge),
         reads=["iot"], writes=["tri01"])
    S.op("dve", lambda e: e.tensor_scalar(out=triNEG[:], in0=iot[:], scalar1=0.0, scalar2=NEG, op0=ALU.is_gt, op1=ALU.mult),
         reads=["iot"], writes=["triNEG"])

    with ExitStack() as es:
        def TT(name, shape, dt):
            return es.enter_context(nc.sbuf_tensor(name, shape, dt))
        posi = TT("posi", [P, NT], I32)
        posf = TT("posf", [P, NT], F32)
        invf = TT("invf", [P, 8], F32)
        ang = TT("ang", [P, NT, 8], F32)
        ang2 = TT("ang2", [P, NT, 8], F32)
        ki_ = TT("ki_", [P, NT, 8], I32)
        kf_ = TT("kf_", [P, NT, 8], F32)
        m1_ = TT("m1_", [P, NT, 8], F32)
        lamt = TT("lamt", [P, 256], F32)
        lj = TT("lj", [P, 64], F32)
        s01 = TT("s01", [P, 2], F32)

        S.dma("sp", lambda e: e.dma_start(out=posi[:], in_=pos_d), "setup", writes=["posi"])
        S.dma("sp", lambda e: e.dma_start(out=lamt[:], in_=lam_d.partition_broadcast(P)), "setup", writes=["lamt"])
        S.dma("sp", lambda e: e.dma_start(out=subg08[:], in_=subg_d.partition_broadcast(P)), "setup", writes=["subg08"])
        S.op("dve", lambda e: e.tensor_copy(out=posf[:], in_=posi[:]), reads=["posi"], writes=["posf"])
        inv = (np.float32(500000.0) ** (-(np.arange(8, dtype=np.float32) * np.float32(2.0)) / np.float32(16.0))).astype(np.float32)
        for i in range(8):
            S.op("dve", lambda e: e.memset(invf[:, i:i + 1], float(inv[i])), writes=["invf"])
        S.op("dve", lambda e: e.tensor_tensor(out=ang[:], in0=posf[:].unsqueeze(2).to_broadcast([P, NT, 8]),
                                              in1=invf[:].unsqueeze(1).to_broadcast([P, NT, 8]), op=ALU.mult),
             reads=["posf", "invf"], writes=["ang"])
        S.op("dve", lambda e: e.tensor_scalar(out=ang2[:], in0=ang[:], scalar1=PI / 2, scalar2=None, op0=ALU.add),
             reads=["ang"], writes=["ang2"])
        C1 = 6.28125
        C2 = float(2 * np.pi - 6.28125)

        def sin_of(a, key):
            S.op("dve", lambda e: e.tensor_scalar(out=ki_[:], in0=a[:], scalar1=float(1 / (2 * np.pi)), scalar2=None, op0=ALU.mult),
                 reads=[key], writes=["ki_"])
            S.op("dve", lambda e: e.tensor_copy(out=kf_[:], in_=ki_[:]), reads=["ki_"], writes=["kf_"])
            S.op("dve", lambda e: e.scalar_tensor_tensor(out=a[:], in0=kf_[:], scalar=-C1, in1=a[:], op0=ALU.mult, op1=ALU.add),
                 reads=["kf_", key], writes=[key])
            S.op("dve", lambda e: e.scalar_tensor_tensor(out=a[:], in0=kf_[:], scalar=-C2, in1=a[:], op0=ALU.mult, op1=ALU.add),
                 reads=["kf_", key], writes=[key])
            S.op("dve", lambda e: e.tensor_scalar(out=m1_[:], in0=a[:], scalar1=PI, scalar2=-2 * PI, op0=ALU.is_gt, op1=ALU.mult),
                 reads=[key], writes=["m1_"])
            S.op("dve", lambda e: e.tensor_tensor(out=a[:], in0=a[:], in1=m1_[:], op=ALU.add), reads=[key, "m1_"], writes=[key])
            S.op("dve", lambda e: e.tensor_scalar(out=m1_[:], in0=a[:], scalar1=-PI, scalar2=2 * PI, op0=ALU.is_lt, op1=ALU.mult),
                 reads=[key], writes=["m1_"])
            S.op("dve", lambda e: e.tensor_tensor(out=a[:], in0=a[:], in1=m1_[:], op=ALU.add), reads=[key, "m1_"], writes=[key])
            S.op("act", lambda e: e.activation(out=a[:], in_=a[:], func=AF.Sin), reads=[key], writes=[key])

        sin_of(ang, "ang")
        sin_of(ang2, "ang2")
        S.op("dve", lambda e: e.tensor_copy(out=CC[:, :, 0:8], in_=ang2[:]), reads=["ang2"], writes=["CC"])
        S.op("dve", lambda e: e.tensor_copy(out=CC[:, :, 8:16], in_=ang2[:]), reads=["ang2"], writes=["CC"])
        S.op("dve", lambda e: e.tensor_scalar(out=SSt[:, :, 0:8], in0=ang[:], scalar1=-1.0, scalar2=None, op0=ALU.mult),
             reads=["ang"], writes=["SSt"])
        S.op("dve", lambda e: e.tensor_copy(out=SSt[:, :, 8:16], in_=ang[:]), reads=["ang"], writes=["SSt"])
        S.op("dve", lambda e: e.scalar_tensor_tensor(out=lj[:], in0=lamt[:, 0:64], scalar=1.0, in1=lamt[:, 64:128],
                                                     op0=ALU.mult, op1=ALU.mult, accum_out=s01[:, 0:1]),
             reads=["lamt"], writes=["lj", "s01"])
        S.op("dve", lambda e: e.scalar_tensor_tensor(out=lj[:], in0=lamt[:, 128:192], scalar=1.0, in1=lamt[:, 192:256],
                                                     op0=ALU.mult, op1=ALU.mult, accum_out=s01[:, 1:2]),
             reads=["lamt", "lj", "s01"], writes=["lj", "s01"])
        S.op("act", lambda e: e.activation(out=s01[:], in_=s01[:], func=AF.Exp), reads=["s01"], writes=["s01"])
        S.op("dve", lambda e: e.tensor_tensor(out=neglam[:], in0=s01[:, 1:2], in1=s01[:, 0:1], op=ALU.subtract),
             reads=["s01"], writes=["neglam"])
        S.op("dve", lambda e: e.tensor_scalar(out=neglam[:], in0=neglam[:], scalar1=-0.2, scalar2=None, op0=ALU.add),
             reads=["neglam"], writes=["neglam"])
        S.op("dve", lambda e: e.tensor_scalar(out=subg08[:], in0=subg08[:], scalar1=0.8, scalar2=None, op0=ALU.mult),
             reads=["subg08"], writes=["subg08"])
        S.barrier()

    def load_w_bf16(dst_ap, src_ap, key):
        S.dma("pool", lambda e: e.dma_start(out=dst_ap, in_=src_ap), "wload", writes=[key])

    def rmsnorm_tile(xt_ap, xkey, gain, hn_ap, hnkey, sfx, junk, ssq, rstd, extra_w=(), gkey="gains"):
        S.op("act", lambda e: e.activation(out=junk[:], in_=xt_ap, func=AF.Square, accum_out=ssq[:]),
             reads=[xkey], writes=["junk" + sfx, "ssq" + sfx])
        S.op("act", lambda e: e.activation(out=rstd[:], in_=ssq[:], func=AF.Sqrt, scale=1.0 / D, bias=EPS),
             reads=["ssq" + sfx], writes=["rstd" + sfx])
        S.op("dve", lambda e: e.reciprocal(out=rstd[:], in_=rstd[:]), reads=["rstd" + sfx], writes=["rstd" + sfx])
        S.op("dve", lambda e: e.scalar_tensor_tensor(out=hn_ap, in0=xt_ap, scalar=rstd[:], in1=gain[:],
                                                     op0=ALU.mult, op1=ALU.mult),
             reads=[xkey, "rstd" + sfx, gkey], writes=[hnkey] + list(extra_w))

    def transpose_to(src_tile, skey, nchunk, bank, dst_ap, dkey, eng="act"):
        for c in range(nchunk):
            S.op("pe", lambda e: e.transpose(out=psbf(bank)[:, c * 128:(c + 1) * 128],
                                             in_=src_tile[:, c * 128:(c + 1) * 128], identity=ident[:]),
                 reads=[skey, "ident"], writes=pk(bank))
        if eng == "act":
            S.op("act", lambda e: e.copy(out=dst_ap, in_=psbf(bank)[:, 0:nchunk * 128]), reads=pk(bank), writes=[dkey])
        else:
            S.op("dve", lambda e: e.tensor_copy(out=dst_ap, in_=psbf(bank)[:, 0:nchunk * 128]), reads=pk(bank), writes=[dkey])

    def proj(lhsT_tile, lkey, nk, W, wkey, c0, n, bank, boff=0):
        for c in range(nk):
            S.op("pe", lambda e: e.matmul(PS[:, bank, boff:boff + n], lhsT=lhsT_tile[:, c, :], rhs=W[:, c, c0:c0 + n],
                                          start=(c == 0), stop=(c == nk - 1)),
                 reads=[lkey, wkey], writes=pk(bank))

    def rope4(src3, dst3, nh, t, tA, tB, rkeys, dkey):
        cc = CC[:, t, :].unsqueeze(1).to_broadcast([P, nh, 16])
        s_lo = SSt[:, t, 0:8].unsqueeze(1).to_broadcast([P, nh, 8])
        s_hi = SSt[:, t, 8:16].unsqueeze(1).to_broadcast([P, nh, 8])
        S.op("act", lambda e: e.copy(out=ropeS[:, 0:nh, :], in_=src3[:, :, 0:16]), reads=rkeys, writes=["ropeS"])
        S.op("dve", lambda e: e.tensor_tensor(out=tA[:, 0:nh, :], in0=ropeS[:, 0:nh, :], in1=cc, op=ALU.mult),
             reads=["ropeS"], writes=["ropeA"])
        S.op("dve", lambda e: e.tensor_tensor(out=tB[:, 0:nh, 0:8], in0=ropeS[:, 0:nh, 8:16], in1=s_lo, op=ALU.mult),
             reads=["ropeS"], writes=["ropeB"])
        S.op("dve", lambda e: e.tensor_tensor(out=tB[:, 0:nh, 8:16], in0=ropeS[:, 0:nh, 0:8], in1=s_hi, op=ALU.mult),
             reads=["ropeS", "ropeB"], writes=["ropeB"])
        S.op("dve", lambda e: e.tensor_tensor(out=dst3[:, :, 0:16], in0=tA[:, 0:nh, :], in1=tB[:, 0:nh, :], op=ALU.add),
             reads=["ropeA", "ropeB", dkey], writes=[dkey])

    kv = ExitStack()

    def KVT(name, shape, dt):
        return kv.enter_context(nc.sbuf_tensor(name, shape, dt))

    KT_da = KVT("KT_da", [P, 4, SL], BF16)
    V_da = KVT("V_da", [P, NT, 4, 130], BF16)
    KT_ds = KVT("KT_ds", [P, SL], BF16)
    V_ds = KVT("V_ds", [P, NT, 2, 66], BF16)
    KT_ix = KVT("KT_ix", [P, SL], BF16)
    ropeA = KVT("ropeA", [P, 24, 16], F32)
    ropeB = KVT("ropeB", [P, 24, 16], F32)
    ropeS = KVT("ropeS", [P, 24, 16], F32)
    Wq = KVT("Wq", [P, KC, 1544], BF16)

    S.op("pool", lambda e: e.memset(V_da[:, :, :, 128:130], 1.0), writes=["Vda"])
    S.op("pool", lambda e: e.memset(V_ds[:, :, :, 64:66], 1.0), writes=["Vds"])

    with ExitStack() as es:
        def TT(name, shape, dt):
            return es.enter_context(nc.sbuf_tensor(name, shape, dt))
        Wkv = TT("Wkv", [P, KC, 1344], BF16)
        gmix = TT("gmix", [P, D], F32)
        xt = [TT("xtA%d" % i, [P, D], F32) for i in range(2)]
        junk = TT("junkA", [P, D], BF16)
        ssq = [TT("ssqA%d" % i, [P, 1], F32) for i in range(2)]
        rstd = [TT("rstdA%d" % i, [P, 1], F32) for i in range(2)]
        hn = [TT("hnA%d" % i, [P, D], BF16) for i in range(2)]
        hnT = [TT("hnTA%d" % i, [P, KC, 128], BF16) for i in range(2)]
        krAll = TT("krAll", [P, 704], BF16)

        w3 = w_in_d.rearrange("(c p) n -> p c n", p=P)
        load_w_bf16(Wkv[:, :, 0:512], w3[:, :, C_DAK:C_DAK + 512], "Wkv")
        load_w_bf16(Wkv[:, :, 512:640], w3[:, :, C_DSK:C_DSK + 128], "Wkv")
        load_w_bf16(Wkv[:, :, 640:704], w3[:, :, C_IXK:C_IXK + 64], "Wkv")
        load_w_bf16(Wkv[:, :, 704:832], w3[:, :, C_DSV:C_DSV + 128], "Wkv")
        load_w_bf16(Wkv[:, :, 832:1344], w3[:, :, C_DAV:C_DAV + 512], "Wkv")
        S.dma("sp", lambda e: e.dma_start(out=gmix[:], in_=g_mix_d.partition_broadcast(P)), "setup", writes=["gains"])
        load_w_bf16(Wq[:, :, 0:512], w3[:, :, C_DAQ:C_DAQ + 512], "Wq")
        load_w_bf16(Wq[:, :, 512:1024], w3[:, :, C_DSQ:C_DSQ + 512], "Wq")
        load_w_bf16(Wq[:, :, 1024:1536], w3[:, :, C_IXQ:C_IXQ + 512], "Wq")
        load_w_bf16(Wq[:, :, 1536:1544], w3[:, :, C_IXW:C_IXW + 8], "Wq")
        for (src_, a_) in ((pu_d, 0), (pv_d, 1)):
            for r0 in range(0, 16384, 4096):
                S.dma("pool", lambda e: e.dma_start(out=puv_d[r0:r0 + 4096, a_, :], in_=src_[r0:r0 + 4096, :]), "tabcast")


        def a_part1(t):
            b = t % 2
            S.dma("sp", lambda e: e.dma_start(out=xt[b][:], in_=xT[t]), "xld", writes=["xt%d" % b])
            rmsnorm_tile(xt[b][:], "xt%d" % b, gmix, hn[b][:], "hn%d" % b, "A%d" % b, junk, ssq[b], rstd[b])
            transpose_to(hn[b], "hn%d" % b, KC, 0, hnT[b][:].rearrange("p c t -> p (c t)"), "hnT%d" % b)
            S.dma("sp", lambda e: e.dma_start(out=hnT_d[t], in_=hnT[b][:].rearrange("p c t -> p (c t)")), "hnTst",
                  reads=["hnT%d" % b], writes=["hnT_d%d" % t])

        def a_part2(t):
            b = t % 2
            B1 = 1 + 4 * b
            proj(hnT[b], "hnT%d" % b, KC, Wkv, "Wkv", 0, 512, B1)
            proj(hnT[b], "hnT%d" % b, KC, Wkv, "Wkv", 512, 320, B1 + 1)
            proj(hnT[b], "hnT%d" % b, KC, Wkv, "Wkv", 832, 512, B1 + 2)
            flat12 = PS[:, B1:B1 + 2, :].rearrange("p a b -> p (a b)")
            S.op("act", lambda e: e.copy(out=krAll[:], in_=flat12[:, 0:704]), reads=pk(B1, B1 + 1), writes=["krAll"])
            rope4(flat12[:, 0:704].rearrange("p (h d) -> p h d", d=64), krAll[:].rearrange("p (h d) -> p h d", d=64),
                  11, t, ropeA, ropeB, pk(B1, B1 + 1) + ["CC"], "krAll")
            S.op("act", lambda e: e.copy(out=V_ds[:, t, :, 0:64], in_=PS[:, B1 + 1, 192:320].rearrange("p (g d) -> p g d", d=64)),
                 reads=pk(B1 + 1), writes=["Vds"])
            S.op("act", lambda e: e.copy(out=V_da[:, t, :, 0:128], in_=PS[:, B1 + 2, :].rearrange("p (h d) -> p h d", d=128)),
                 reads=pk(B1 + 2), writes=["Vda"])
            for j in range(5):
                S.op("pe", lambda e: e.transpose(out=psbf(4)[:, j * 128:(j + 1) * 128], in_=krAll[:, j * 128:(j + 1) * 128],
                                                 identity=ident[:]), reads=["krAll", "ident"], writes=pk(4))
            S.op("pe", lambda e: e.transpose(out=psbf(4)[0:64, 640:768], in_=krAll[:, 640:704], identity=ident[:]),
                 reads=["krAll", "ident"], writes=pk(4))
            tok = slice(t * 128, (t + 1) * 128)
            S.op("dve", lambda e: e.tensor_copy(out=KT_da[:, :, tok], in_=psbf(4)[:, 0:512].rearrange("p (h t) -> p h t", t=128)),
                 reads=pk(4), writes=["KTda"])
            S.op("dve", lambda e: e.tensor_copy(out=KT_ds[:, tok], in_=psbf(4)[:, 512:640]), reads=pk(4), writes=["KTds"])
            S.op("dve", lambda e: e.tensor_copy(out=KT_ix[0:64, tok], in_=psbf(4)[0:64, 640:768]), reads=pk(4), writes=["KTix"])

        a_part1(0)
        for t in range(NT):
            if t + 1 < NT:
                a_part1(t + 1)
            a_part2(t)
        S.barrier()

    if debug:
        for hh in range(4):
            S.dma("sp", lambda e: e.dma_start(out=dbg["kt"][:, hh * SL:(hh + 1) * SL], in_=KT_da[:, hh, :]), "dbg", reads=["KTda"])

    if stop_after != "A":
      with ExitStack() as es:
        def TT(name, shape, dt):
            return es.enter_context(nc.sbuf_tensor(name, shape, dt))
        hnT = [TT("hnTB%d" % i, [P, KC, 128], BF16) for i in range(1)] * 2
        qall = TT("qall", [P, 1536], BF16)
        QP_ds = TT("QP_ds", [P, 8, 128], BF16)
        QT_da2 = [TT("QT_da%d" % i, [P, 4, 512], BF16) for i in range(2)]
        QT_ds2 = [TT("QT_ds%d" % i, [P, 8, 128], BF16) for i in range(2)]
        QIT = TT("QIT", [P, 8, 128], BF16)
        ixw = TT("ixw", [P, 8], F32)
        absw = TT("absw", [P, 8], F32)
        sgn = TT("sgn", [P, 8], F32)
        rtmp = [TT("rtmp%d" % i, [P, 512], F32) for i in range(2)]
        idx = TT("idx", [P, SL], F32)
        mneg = TT("mneg", [P, SL], BF16)
        maskT2 = [TT("maskT%d" % i, [P, NT, 128], BF16) for i in range(2)]
        PTd = [TT("PTd%d" % i, [P, 8, 128], BF16) for i in range(2)]
        PTa = [TT("PTa%d" % i, [P, 2, 512], BF16) for i in range(2)]
        bl = TT("bl", [P, 8], F32)
        wk = TT("wk", [P, NBIS], F32)
        fvec = TT("fvec", [P, NBIS], F32)
        rcb = TT("rcb", [P, 8], F32)
        o_b = TT("o_b", [P, 512], BF16)
        obT = TT("obT", [P, 4, 128], BF16)
        o_a = TT("o_a", [P, 4, 512], BF16)
        oaT = [TT("oaT%d" % i, [P, 4, 128], BF16) for i in range(2)]
        od = TT("od", [P, 4, 128], F32)
        rca = TT("rca", [P, 8], F32)
        rcl = TT("rcl", [P, 4], F32)
        ssa = TT("ssa", [P, 4], F32)
        junkb = TT("junkb", [P, 128], BF16)

        w3 = w_in_d.rearrange("(c p) n -> p c n", p=P)
        for it_ in range(NBIS):
            S.op("pool", lambda e: e.memset(fvec[:, it_:it_ + 1], 0.5 ** (it_ + 1)), writes=["fvec"])
        S.op("pool", lambda e: e.memset(QP_ds[:], 0.0), writes=["QP_ds"])
        S.op("pool", lambda e: e.memset(QIT[:], 0.0), writes=["QIT"])

        LO, HI, W0, MID, CNT, SELW, THR = range(7)

        def col(i):
            return bl[:, i:i + 1]

        def dsa_tile(qt, hook=None):
            maskT = maskT2[qt % 2]
            mkey = "maskT%d" % (qt % 2)
            kend = (qt + 1) * 128
            nkb = qt + 1
            nch = (kend + 511) // 512
            for c in range(nch):
                n = min(512, kend - c * 512)
                accb = 2 + (c % 2)
                for h in range(8):
                    pb = h % 2
                    S.op("pe", lambda e: e.matmul(PS[:, pb, 0:n], lhsT=QIT[0:64, h, :], rhs=KT_ix[0:64, c * 512:c * 512 + n],
                                                  start=True, stop=True),
                         reads=["QIT", "KTix"], writes=pk(pb))
                    S.op("act", lambda e: e.activation(out=rtmp[pb][:, 0:n], in_=PS[:, pb, 0:n], func=AF.Relu,
                                                       scale=absw[:, h:h + 1]),
                         reads=pk(pb) + ["absw"], writes=["rtmp%d" % pb])
                    if h == 0:
                        S.op("dve", lambda e: e.tensor_scalar(out=PS[:, accb, 0:n], in0=rtmp[pb][:, 0:n], scalar1=sgn[:, 0:1],
                                                              scalar2=None, op0=ALU.mult),
                             reads=["rtmp%d" % pb, "sgn"], writes=pk(accb))
                    else:
                        S.op("dve", lambda e: e.scalar_tensor_tensor(out=PS[:, accb, 0:n], in0=rtmp[pb][:, 0:n],
                                                                     scalar=sgn[:, h:h + 1], in1=PS[:, accb, 0:n],
                                                                     op0=ALU.mult, op1=ALU.add),
                             reads=["rtmp%d" % pb, "sgn"] + pk(accb), writes=pk(accb))
                last = (c == nch - 1)
                n0 = n - 128 if last else n
                if n0 > 0:
                    S.op("act", lambda e: e.copy(out=idx[:, c * 512:c * 512 + n0], in_=PS[:, accb, 0:n0]),
                         reads=pk(accb), writes=["idx"])
                if last:
                    S.op("dve", lambda e: e.tensor_tensor(out=idx[:, kend - 128:kend], in0=PS[:, accb, n0:n], in1=triNEG[:],
                                                          op=ALU.add),
                         reads=pk(accb) + ["triNEG"], writes=["idx"])
            if qt >= 2:
                S.op("dve", lambda e: e.tensor_reduce(out=col(HI), in_=idx[:, 0:kend], axis=AX.X, op=ALU.max),
                     reads=["idx"], writes=["bl"])
                S.op("dve", lambda e: e.tensor_reduce(out=col(LO), in_=idx[:, 0:kend - 128], axis=AX.X, op=ALU.min),
                     reads=["idx", "bl"], writes=["bl"])
                S.op("dve", lambda e: e.tensor_tensor(out=col(W0), in0=col(HI), in1=col(LO), op=ALU.subtract),
                     reads=["bl"], writes=["bl"])
                S.op("dve", lambda e: e.tensor_scalar(out=wk[:], in0=fvec[:], scalar1=col(W0), scalar2=None, op0=ALU.mult),
                     reads=["bl", "fvec"], writes=["wk"])
                S.op("dve", lambda e: e.tensor_tensor(out=col(MID), in0=col(LO), in1=wk[:, 0:1], op=ALU.add),
                     reads=["bl", "wk"], writes=["bl"])
                for it in range(NBIS):
                    S.op("dve", lambda e: e.tensor_scalar(out=mneg[:, 0:kend], in0=idx[:, 0:kend], scalar1=col(MID),
                                                          scalar2=None, op0=ALU.is_ge, op1=ALU.add, accum_out=col(CNT)),
                         reads=["idx", "bl"], writes=["mneg", "bl"])
                    S.op("dve", lambda e: e.tensor_scalar(out=col(SELW), in0=col(CNT), scalar1=float(NSEL) - 0.5, scalar2=0.5,
                                                          op0=ALU.is_ge, op1=ALU.subtract),
                         reads=["bl"], writes=["bl"])
                    if it < NBIS - 1:
                        S.op("dve", lambda e: e.scalar_tensor_tensor(out=col(MID), in0=col(SELW), scalar=wk[:, it:it + 1], in1=col(MID),
                                                                     op0=ALU.mult, op1=ALU.add),
                             reads=["bl", "wk"], writes=["bl"])
                S.op("dve", lambda e: e.tensor_scalar(out=col(SELW), in0=col(SELW), scalar1=-0.5, scalar2=None, op0=ALU.add),
                     reads=["bl"], writes=["bl"])
                S.op("dve", lambda e: e.scalar_tensor_tensor(out=col(LO), in0=col(SELW), scalar=wk[:, NBIS - 1:NBIS], in1=col(MID),
                                                             op0=ALU.mult, op1=ALU.add),
                     reads=["bl", "wk"], writes=["bl"])
                thr = col(LO)
            else:
                S.op("dve", lambda e: e.memset(col(THR), -1.0e29), reads=["bl"], writes=["bl"])
                thr = col(THR)
            S.op("dve", lambda e: e.tensor_scalar(out=mneg[:, 0:kend], in0=idx[:, 0:kend], scalar1=thr, scalar2=NEG,
                                                  op0=ALU.is_lt, op1=ALU.mult),
                 reads=["idx", "bl"], writes=["mneg"])
            if hook is not None:
                hook()
            for k0 in range(0, nkb, 8):
                k1 = min(nkb, k0 + 8)
                tb = 3 - ((k0 // 8) % 2)
                for kb in range(k0, k1):
                    S.op("pe", lambda e: e.transpose(out=psbf(tb)[:, (kb - k0) * 128:(kb - k0 + 1) * 128],
                                                     in_=mneg[:, kb * 128:(kb + 1) * 128], identity=ident[:]),
                         reads=["mneg", "ident"], writes=pk(tb))
                S.op("act", lambda e: e.copy(out=maskT[:, k0:k1, :].rearrange("p k q -> p (k q)"),
                                             in_=psbf(tb)[:, 0:(k1 - k0) * 128]),
                     reads=pk(tb), writes=[mkey])

        def dsa_part2(qt):
            kend = (qt + 1) * 128
            nkb = qt + 1
            maskT = maskT2[qt % 2]
            mkey = "maskT%d" % (qt % 2)
            QT_ds = QT_ds2[qt % 2]
            qdkey = "QT_ds%d" % (qt % 2)
            def dsa_scores(kb):
                sbk = 4 if kb % 2 == 0 else 2
                for g in range(2):
                    S.op("pe", lambda e: e.matmul(PS[:, sbk + g, :], lhsT=KT_ds[:, kb * 128:(kb + 1) * 128],
                                                  rhs=QT_ds[:, 4 * g:4 * g + 4, :].rearrange("p h q -> p (h q)"),
                                                  start=True, stop=False),
                         reads=["KTds", qdkey], writes=pk(sbk + g))
                    S.op("pe", lambda e: e.matmul(PS[:, sbk + g, :].rearrange("p (h q) -> p h q", h=4), lhsT=ident[:],
                                                  rhs=maskT[:, kb, :].unsqueeze(1).to_broadcast([P, 4, 128]), start=False, stop=True),
                         reads=["ident", mkey], writes=pk(sbk + g))
            dsa_scores(0)
            for kb in range(nkb):
                pb2 = kb % 2
                sbk = 4 if kb % 2 == 0 else 2
                if kb + 1 < nkb:
                    dsa_scores(kb + 1)
                S.op("act", lambda e: e.activation(out=PTd[pb2][:].rearrange("p h q -> p (h q)"),
                                                   in_=PS[:, sbk:sbk + 2, :].rearrange("p a b -> p (a b)"), func=AF.Exp, scale=0.125),
                     reads=pk(sbk, sbk + 1), writes=["PTd%d" % pb2])
                for h in range(8):
                    S.op("pe", lambda e: e.matmul(PS[:, 6 + h // 4, (h % 4) * 66:(h % 4) * 66 + 66], lhsT=PTd[pb2][:, h, :],
                                                  rhs=V_ds[:, kb, h // 4, :], start=(kb == 0 and h % 4 == 0),
                                                  stop=(kb == nkb - 1), skip_group_check=True),
                         reads=["PTd%d" % pb2, "Vds"], writes=pk(6 + h // 4))
            pov = PS[:, 6:8, 0:264].rearrange("p b (h e) -> p b h e", e=66)
            S.op("dve", lambda e: e.reciprocal(out=rcb[:].rearrange("p (b h) -> p b h", b=2), in_=pov[:, :, :, 64]),
                 reads=pk(6, 7), writes=["rcb"])
            S.op("dve", lambda e: e.tensor_tensor(out=o_b[:].rearrange("p (b h d) -> p b h d", b=2, h=4),
                                                  in0=pov[:, :, :, 0:64],
                                                  in1=rcb[:].rearrange("p (b h) -> p b h", b=2).unsqueeze(3).to_broadcast([P, 2, 4, 64]),
                                                  op=ALU.mult),
                 reads=pk(6, 7) + ["rcb"], writes=["o_b"])
            if debug:
                S.dma("sp", lambda e: e.dma_start(out=dbg["ob"][qt], in_=o_b[:]), "dbg", reads=["o_b"])
            transpose_to(o_b, "o_b", 4, 3, obT[:].rearrange("p c t -> p (c t)"), "obT")
            S.dma("sp", lambda e: e.dma_start(out=obT_d[qt], in_=obT[:].rearrange("p c t -> p (c t)")), "obTst",
                  reads=["obT"], writes=["obT_d%d" % qt])

        def diff_head(qs, h):
            nkb = 4 * qs + 4
            QT_da = QT_da2[qs % 2]
            qkey = "QT_da%d" % (qs % 2)
            if True:
                def da_scores(kb):
                    j = kb - 4 * qs
                    q0 = max(0, j) * 128
                    sb = kb % 2
                    for m in range(2):
                        S.op("pe", lambda e: e.matmul(PS[:, 2 * sb + m, q0:512], lhsT=KT_da[64 * m:64 * m + 64, h, kb * 128:(kb + 1) * 128],
                                                      rhs=QT_da[64 * m:64 * m + 64, h, q0:512], start=True, stop=True),
                             reads=["KTda", qkey], writes=pk(2 * sb + m))
                da_scores(0)
                for kb in range(nkb):
                    j = kb - 4 * qs
                    jj = max(0, j)
                    q0 = jj * 128
                    sb = kb % 2
                    if kb + 1 < nkb:
                        da_scores(kb + 1)
                    S.op("act", lambda e: e.activation(out=PTa[sb][:, :, q0:512], in_=PS[:, 2 * sb:2 * sb + 2, q0:512],
                                                       func=AF.Exp, scale=0.125),
                         reads=pk(2 * sb, 2 * sb + 1), writes=["PTa%d" % sb])
                    if j >= 0:
                        S.op("pool", lambda e: e.tensor_tensor(out=PTa[sb][:, :, q0:q0 + 128], in0=PTa[sb][:, :, q0:q0 + 128],
                                                               in1=tri01[:].unsqueeze(1).to_broadcast([P, 2, 128]), op=ALU.mult),
                             reads=["PTa%d" % sb, "tri01"], writes=["PTa%d" % sb])
                    for m in range(2):
                        for qi in range(jj, 4):
                            r = m * 4 + qi
                            S.op("pe", lambda e: e.matmul(PS[:, 4 + r // 2, (r % 2) * 130:(r % 2) * 130 + 130],
                                                          lhsT=PTa[sb][:, m, qi * 128:(qi + 1) * 128], rhs=V_da[:, kb, h, :],
                                                          start=(kb == 0 and r % 2 == 0), stop=(kb == 4 * qs + qi),
                                                          skip_group_check=True),
                                 reads=["PTa%d" % sb, "Vda"], writes=pk(4 + r // 2))
                pov = PS[:, 4:8, 0:260].rearrange("p b (s e) -> p b s e", e=130)
                S.op("dve", lambda e: e.reciprocal(out=rca[:].rearrange("p (b s) -> p b s", s=2), in_=pov[:, :, :, 128]),
                     reads=pk(4, 5, 6, 7), writes=["rca"])
                S.op("dve", lambda e: e.tensor_scalar(out=rcl[:], in0=rca[:, 4:8], scalar1=neglam[:, 0:1], scalar2=None,
                                                      op0=ALU.mult), reads=["rca", "neglam"], writes=["rcl"])
                for qi in range(4):
                    r0, r1 = qi, 4 + qi
                    S.op("dve", lambda e: e.tensor_scalar(out=od[:, qi, :], in0=PS[:, 4 + r0 // 2, (r0 % 2) * 130:(r0 % 2) * 130 + 128],
                                                          scalar1=rca[:, r0:r0 + 1], scalar2=None, op0=ALU.mult),
                         reads=pk(4 + r0 // 2) + ["rca"], writes=["od%d" % qi])
                    S.op("dve", lambda e: e.scalar_tensor_tensor(out=od[:, qi, :],
                                                                 in0=PS[:, 4 + r1 // 2, (r1 % 2) * 130:(r1 % 2) * 130 + 128],
                                                                 scalar=rcl[:, qi:qi + 1], in1=od[:, qi, :], op0=ALU.mult, op1=ALU.add),
                         reads=pk(4 + r1 // 2) + ["rcl", "od%d" % qi], writes=["od%d" % qi])
                    S.op("act", lambda e: e.activation(out=junkb[:], in_=od[:, qi, :], func=AF.Square, accum_out=ssa[:, qi:qi + 1]),
                         reads=["od%d" % qi], writes=["junkb", "ssa%d" % qi])
                S.op("act", lambda e: e.activation(out=ssa[:], in_=ssa[:], func=AF.Sqrt, scale=1.0 / 128, bias=EPS),
                     reads=["ssa%d" % i for i in range(4)], writes=["ssa"])
                S.op("dve", lambda e: e.reciprocal(out=ssa[:], in_=ssa[:]), reads=["ssa"], writes=["ssa"] + ["ssa%d" % i for i in range(4)])
                for qi in range(4):
                    S.op("dve", lambda e: e.scalar_tensor_tensor(out=o_a[:, qi, h * 128:(h + 1) * 128], in0=od[:, qi, :],
                                                                 scalar=ssa[:, qi:qi + 1], in1=subg08[:], op0=ALU.mult, op1=ALU.mult),
                         reads=["od%d" % qi, "ssa", "subg08"], writes=["o_a"])

        def diff_finalize(qs):
            for qi in range(4):
                qt = qs * 4 + qi
                ob_ = oaT[qi % 2]
                if debug:
                    S.dma("sp", lambda e: e.dma_start(out=dbg["oa"][qt], in_=o_a[:, qi, :]), "dbg", reads=["o_a"])
                for hh in range(4):
                    S.op("pe", lambda e: e.transpose(out=psbf(qi % 2)[:, hh * 128:(hh + 1) * 128],
                                                     in_=o_a[:, qi, hh * 128:(hh + 1) * 128], identity=ident[:]),
                         reads=["o_a", "ident"], writes=pk(qi % 2))
                S.op("act", lambda e: e.copy(out=ob_[:].rearrange("p c t -> p (c t)"), in_=psbf(qi % 2)[:, 0:512]),
                     reads=pk(qi % 2), writes=["oaT%d" % (qi % 2)])
                S.dma("sp", lambda e: e.dma_start(out=oaT_d[qt], in_=ob_[:].rearrange("p c t -> p (c t)")), "oaTst",
                      reads=["oaT%d" % (qi % 2)], writes=["oaT_d%d" % qt])

        nqs = 8

        def qproj(qt):
            qs, qi = qt // 4, qt % 4
            b = qt % 2
            S.dma("sp", lambda e: e.dma_start(out=hnT[b][:].rearrange("p c t -> p (c t)"), in_=hnT_d[qt]), "hnTld",
                  reads=["hnT_d%d" % qt], writes=["hnTB"])
            proj(hnT[b], "hnTB", KC, Wq, "Wq", 0, 512, 0)
            proj(hnT[b], "hnTB", KC, Wq, "Wq", 512, 512, 1)
            proj(hnT[b], "hnTB", KC, Wq, "Wq", 1024, 512, 2)
            proj(hnT[b], "hnTB", KC, Wq, "Wq", 1536, 8, 3)
            flat = PS[:, 0:3, :].rearrange("p a b -> p (a b)")
            S.op("act", lambda e: e.copy(out=qall[:], in_=flat), reads=pk(0, 1, 2), writes=["qall"])
            rope4(flat.rearrange("p (h d) -> p h d", d=64), qall[:].rearrange("p (h d) -> p h d", d=64), 24, qt,
                  ropeA, ropeB, pk(0, 1, 2) + ["CC"], "qall")
            S.op("dve", lambda e: e.tensor_copy(out=ixw[:], in_=PS[:, 3, 0:8]), reads=pk(3), writes=["ixw"])
            S.op("dve", lambda e: e.scalar_tensor_tensor(out=absw[:], in0=ixw[:], scalar=-1.0, in1=ixw[:], op0=ALU.mult, op1=ALU.max),
                 reads=["ixw"], writes=["absw"])
            S.op("dve", lambda e: e.tensor_scalar(out=sgn[:], in0=ixw[:], scalar1=0.0, scalar2=2.0, op0=ALU.is_ge, op1=ALU.mult),
                 reads=["ixw"], writes=["sgn"])
            S.op("dve", lambda e: e.tensor_scalar(out=sgn[:], in0=sgn[:], scalar1=-1.0, scalar2=None, op0=ALU.add),
                 reads=["sgn"], writes=["sgn"])
            S.op("pool", lambda e: e.tensor_copy(out=QP_ds[:, 0:4, 0:64], in_=qall[:, 512:768].rearrange("p (h d) -> p h d", d=64)),
                 reads=["qall"], writes=["QP_ds"])
            S.op("pool", lambda e: e.tensor_copy(out=QP_ds[:, 4:8, 64:128], in_=qall[:, 768:1024].rearrange("p (h d) -> p h d", d=64)),
                 reads=["qall", "QP_ds"], writes=["QP_ds"])
            for j in range(4):
                S.op("pe", lambda e: e.transpose(out=psbf(4)[:, j * 128:(j + 1) * 128], in_=qall[:, j * 128:(j + 1) * 128],
                                                 identity=ident[:]), reads=["qall", "ident"], writes=pk(4))
            for h in range(8):
                S.op("pe", lambda e: e.transpose(out=psbf(5)[:, h * 128:(h + 1) * 128], in_=QP_ds[:, h, :], identity=ident[:]),
                     reads=["QP_ds", "ident"], writes=pk(5))
            for h in range(8):
                S.op("pe", lambda e: e.transpose(out=psbf(6)[0:64, h * 128:(h + 1) * 128],
                                                 in_=qall[:, 1024 + h * 64:1024 + (h + 1) * 64], identity=ident[:]),
                     reads=["qall", "ident"], writes=pk(6))
            S.op("act", lambda e: e.copy(out=QT_da2[qs % 2][:, :, qi * 128:(qi + 1) * 128],
                                         in_=psbf(4)[:, 0:512].rearrange("p (h t) -> p h t", t=128)),
                 reads=pk(4), writes=["QT_da%d" % (qs % 2)])
            S.op("dve", lambda e: e.tensor_copy(out=QT_ds2[qt % 2][:].rearrange("p h t -> p (h t)"), in_=psbf(5)[:, :]),
                 reads=pk(5), writes=["QT_ds%d" % (qt % 2)])
            S.op("act", lambda e: e.copy(out=QIT[0:64, :, :].rearrange("p h t -> p (h t)"), in_=psbf(6)[0:64, :]),
                 reads=pk(6), writes=["QIT"])

        qproj(0)
        for qs in range(nqs):
            for qi in range(4):
                qt = qs * 4 + qi
                def hook(qs=qs, qi=qi, qt=qt):
                    if qt >= 1:
                        dsa_part2(qt - 1)
                    if qs >= 1:
                        diff_head(qs - 1, qi)
                    if qt + 1 < 4 * nqs:
                        qproj(qt + 1)
                dsa_tile(qt, hook=hook)
            if qs >= 1:
                diff_finalize(qs - 1)
        dsa_part2(4 * nqs - 1)
        for h in range(4):
            diff_head(nqs - 1, h)
        diff_finalize(nqs - 1)
        S.barrier()
    kv.close()

    if stop_after not in ("A", "B"):
      with ExitStack() as es:
        def TT(name, shape, dt):
            return es.enter_context(nc.sbuf_tensor(name, shape, dt))
        Wg = TT("Wg", [P, KC, 2048], BF16)
        Wa = TT("Wa", [P, 4, D], BF16)
        Wb = TT("Wb", [P, 4, D], BF16)
        Wo = TT("Wo", [P, KC, D], BF16)
        Wmq = TT("Wmq", [P, KC, 512], BF16)
        Wmo = TT("Wmo", [P, 4, D], BF16)
        gbp = TT("gbp", [P, 2048], BF16)
        onesp = TT("onesp", [P, P], BF16)
        gmem = TT("gmem", [P, D], F32)
        KmT = TT("KmT", [P, 4, 256], BF16)
        Vm = TT("Vm", [P, 2, 4, 130], BF16)

        load_w_bf16(Wg[:], w_in_d.rearrange("(c p) n -> p c n", p=P)[:, :, C_GATE:C_GATE + 2048], "Wg")
        load_w_bf16(Wa[:], wa_d.rearrange("(c p) n -> p c n", p=P), "Wa")
        load_w_bf16(Wb[:], wb_d.rearrange("(c p) n -> p c n", p=P), "Wb")
        load_w_bf16(Wo[:], wo_d.rearrange("(c p) n -> p c n", p=P), "Wo")
        load_w_bf16(Wmq[:], wmq_d.rearrange("(c p) n -> p c n", p=P), "Wmq")
        load_w_bf16(Wmo[:], wmo_d.rearrange("(c p) n -> p c n", p=P), "Wmo")
        S.op("pool", lambda e: e.memset(gbp[:], 0.0), writes=["gbp"])
        S.dma("pool", lambda e: e.dma_start(out=gbp[0:1, :], in_=gbias_d.rearrange("(o n) -> o n", o=1)), "wload",
              reads=["gbp"], writes=["gbp"])
        S.op("pool", lambda e: e.memset(onesp[:], 0.0), writes=["onesp"])
        S.op("pool", lambda e: e.memset(onesp[0:1, :], 1.0), reads=["onesp"], writes=["onesp"])
        S.dma("sp", lambda e: e.dma_start(out=gmem[:], in_=g_mem_d.partition_broadcast(P)), "setup", writes=["gains"])
        S.op("pool", lambda e: e.memset(Vm[:, :, :, 128:130], 1.0), writes=["Vm"])

        with ExitStack() as es2:
            def T2(name, shape, dt):
                return es2.enter_context(nc.sbuf_tensor(name, shape, dt))
            Wmkv = T2("Wmkv", [P, KC, D], BF16)
            gkv = T2("gkv", [P, D], F32)
            mt = T2("mt", [P, D], F32)
            junk0 = T2("junk0", [P, D], BF16)
            ssq0 = T2("ssq0", [P, 1], F32)
            rstd0 = T2("rstd0", [P, 1], F32)
            hm0 = T2("hm0", [P, D], BF16)
            hmT0 = T2("hmT0", [P, KC, 128], BF16)
            load_w_bf16(Wmkv[:], wmkv_d.rearrange("(c p) n -> p c n", p=P), "Wmkv")
            S.dma("sp", lambda e: e.dma_start(out=gkv[:], in_=g_kv_d.partition_broadcast(P)), "setup", writes=["gainkv"])
            memT = mem_d.rearrange("(t p) d -> t p d", p=P)
            for mb in range(2):
                S.dma("sp", lambda e: e.dma_start(out=mt[:], in_=memT[mb]), "xld", writes=["mt"])
                rmsnorm_tile(mt[:], "mt", gkv, hm0[:], "hm0", "0", junk0, ssq0, rstd0, gkey="gainkv")
                transpose_to(hm0, "hm0", KC, 0, hmT0[:].rearrange("p c t -> p (c t)"), "hmT0")
                for hh in range(4):
                    for c in range(KC):
                        S.op("pe", lambda e: e.matmul(PS[:, 1, hh * 128:(hh + 1) * 128], lhsT=Wmkv[:, c, hh * 128:(hh + 1) * 128],
                                                      rhs=hmT0[:, c, :], start=(c == 0 and hh == 0), stop=(c == KC - 1),
                                                      skip_group_check=True),
                             reads=["Wmkv", "hmT0"], writes=pk(1))
                S.op("act", lambda e: e.copy(out=KmT[:, :, mb * 128:(mb + 1) * 128], in_=PS[:, 1, :].rearrange("p (h m) -> p h m", m=128)),
                     reads=pk(1), writes=["KmT"])
                proj(hmT0, "hmT0", KC, Wmkv, "Wmkv", 512, 512, 2)
                S.op("act", lambda e: e.copy(out=Vm[:, mb, :, 0:128], in_=PS[:, 2, :].rearrange("p (h d) -> p h d", d=128)),
                     reads=pk(2), writes=["Vm"])
            S.barrier()

        def T2x(name, shape, dt):
            return [TT("%s_%d" % (name, i), shape, dt) for i in range(2)]
        xt = T2x("xtC", [P, D], F32)
        hnT = T2x("hnTC", [P, KC, 128], BF16)
        oaT = T2x("oaTC", [P, 4, 128], BF16)
        obT = T2x("obTC", [P, 4, 128], BF16)
        gsig = T2x("gsig", [P, 2048], F32)
        t1 = T2x("t1", [P, D], F32)
        t2 = T2x("t2", [P, D], F32)
        mix = T2x("mix", [P, D], BF16)
        mixT = T2x("mixT", [P, KC, 128], BF16)
        x1 = T2x("x1", [P, D], F32)
        junk = T2x("junkC", [P, D], BF16)
        ssq = T2x("ssqC", [P, 1], F32)
        rstd = T2x("rstdC", [P, 1], F32)
        hm = T2x("hm", [P, D], BF16)
        hmT = T2x("hmT", [P, KC, 128], BF16)
        qmT = T2x("qmT", [P, 4, 128], BF16)
        PTm = T2x("PTm", [P, 8, 128], BF16)
        rcm = T2x("rcm", [P, 4], F32)
        om = T2x("om", [P, 512], BF16)
        omT = T2x("omT", [P, 4, 128], BF16)
        x2 = T2x("x2C", [P, D], F32)

        def c1_steps(t):
            p = t % 2
            B0, B1, B2, B3 = 4 * p, 4 * p + 1, 4 * p + 2, 4 * p + 3
            sx = "_%d" % p
            st = []
            A = st.append

            def loads():
                S.dma("sp", lambda e: e.dma_start(out=xt[p][:], in_=xT[t]), "xld", writes=["xt" + sx])
                S.dma("sp", lambda e: e.dma_start(out=hnT[p][:].rearrange("p c t -> p (c t)"), in_=hnT_d[t]), "hnTld", writes=["hnT" + sx])
                S.dma("sp", lambda e: e.dma_start(out=oaT[p][:].rearrange("p c t -> p (c t)"), in_=oaT_d[t]), "oaTld", writes=["oaT" + sx])
                S.dma("sp", lambda e: e.dma_start(out=obT[p][:].rearrange("p c t -> p (c t)"), in_=obT_d[t]), "obTld", writes=["obT" + sx])
            A(loads)

            def gates(r):
                for gq in (2 * r, 2 * r + 1):
                    bank = (B0, B1, B2, B3)[gq]
                    for c in range(KC):
                        S.op("pe", lambda e: e.matmul(PS[:, bank, :], lhsT=hnT[p][:, c, :], rhs=Wg[:, c, gq * 512:(gq + 1) * 512],
                                                      start=(c == 0), stop=False), reads=["hnT" + sx, "Wg"], writes=pk(bank))
                    S.op("pe", lambda e: e.matmul(PS[:, bank, :], lhsT=onesp[:], rhs=gbp[:, gq * 512:(gq + 1) * 512], start=False, stop=True),
                         reads=["onesp", "gbp"], writes=pk(bank))
                bk = B0 if r == 0 else B2
                S.op("act", lambda e: e.activation(out=gsig[p][:, r * D:(r + 1) * D], in_=PS[:, bk:bk + 2, :].rearrange("p a b -> p (a b)"),
                                                   func=AF.Sigmoid),
                     reads=pk(bk, bk + 1), writes=["gsig%d" % r + sx])
            A(lambda: gates(0))
            A(lambda: gates(1))

            def branch(r):
                W_, src, skey = (Wa, oaT[p], "oaT" + sx) if r == 0 else (Wb, obT[p], "obT" + sx)
                bk = B0 if r == 0 else B2
                for hf in range(2):
                    for c in range(4):
                        S.op("pe", lambda e: e.matmul(PS[:, bk + hf, :], lhsT=src[:, c, :], rhs=W_[:, c, hf * 512:(hf + 1) * 512],
                                                      start=(c == 0), stop=(c == 3)), reads=[skey, "Wa" if r == 0 else "Wb"], writes=pk(bk + hf))
                dst = t1[p] if r == 0 else t2[p]
                S.op("dve", lambda e: e.tensor_tensor(out=dst[:], in0=PS[:, bk:bk + 2, :].rearrange("p a b -> p (a b)"),
                                                      in1=gsig[p][:, r * D:(r + 1) * D], op=ALU.mult),
                     reads=pk(bk, bk + 1) + ["gsig%d" % r + sx], writes=["t%d" % r + sx])
            A(lambda: branch(0))
            A(lambda: branch(1))
            A(lambda: S.op("pool", lambda e: e.tensor_tensor(out=mix[p][:], in0=t1[p][:], in1=t2[p][:], op=ALU.add),
                           reads=["t0" + sx, "t1" + sx], writes=["mix" + sx]))
            A(lambda: transpose_to(mix[p], "mix" + sx, KC, B0, mixT[p][:].rearrange("p c t -> p (c t)"), "mixT" + sx))

            def outproj():
                proj(mixT[p], "mixT" + sx, KC, Wo, "Wo", 0, 512, B1)
                proj(mixT[p], "mixT" + sx, KC, Wo, "Wo", 512, 512, B2)
                S.op("dve", lambda e: e.tensor_tensor(out=x1[p][:], in0=PS[:, B1:B1 + 2, :].rearrange("p a b -> p (a b)"), in1=xt[p][:], op=ALU.add),
                     reads=pk(B1, B2) + ["xt" + sx], writes=["x1" + sx])
                if debug:
                    S.dma("sp", lambda e: e.dma_start(out=dbg["x1"][t], in_=x1[p][:]), "dbg", reads=["x1" + sx])
            A(outproj)
            A(lambda: rmsnorm_tile(x1[p][:], "x1" + sx, gmem, hm[p][:], "hm" + sx, "C" + sx, junk[p], ssq[p], rstd[p]))
            A(lambda: transpose_to(hm[p], "hm" + sx, KC, B3, hmT[p][:].rearrange("p c t -> p (c t)"), "hmT" + sx))

            def qproj():
                for hh in range(4):
                    for c in range(KC):
                        S.op("pe", lambda e: e.matmul(PS[:, B0, hh * 128:(hh + 1) * 128], lhsT=Wmq[:, c, hh * 128:(hh + 1) * 128],
                                                      rhs=hmT[p][:, c, :], start=(c == 0 and hh == 0), stop=(c == KC - 1),
                                                      skip_group_check=True),
                             reads=["Wmq", "hmT" + sx], writes=pk(B0))
                S.op("act", lambda e: e.copy(out=qmT[p][:].rearrange("p h t -> p (h t)"), in_=PS[:, B0, :]), reads=pk(B0), writes=["qmT" + sx])
            A(qproj)

            def scores():
                for hh in range(4):
                    for mb in range(2):
                        r = hh * 2 + mb
                        bank = B1 + r // 4
                        S.op("pe", lambda e: e.matmul(PS[:, bank, (r % 4) * 128:(r % 4 + 1) * 128],
                                                      lhsT=KmT[:, hh, mb * 128:(mb + 1) * 128], rhs=qmT[p][:, hh, :],
                                                      start=(r % 4 == 0), stop=True, skip_group_check=True),
                             reads=["KmT", "qmT" + sx], writes=pk(bank))
                S.op("act", lambda e: e.activation(out=PTm[p][:].rearrange("p r t -> p (r t)"), in_=PS[:, B1:B1 + 2, :].rearrange("p a b -> p (a b)"),
                                                   func=AF.Exp, scale=float(128 ** -0.5)),
                     reads=pk(B1, B2), writes=["PTm" + sx])
            A(scores)

            def pv():
                for hh in range(4):
                    bank = B3 if hh < 2 else B0
                    for mb in range(2):
                        S.op("pe", lambda e: e.matmul(PS[:, bank, (hh % 2) * 130:(hh % 2) * 130 + 130],
                                                      lhsT=PTm[p][:, hh * 2 + mb, :], rhs=Vm[:, mb, hh, :],
                                                      start=(mb == 0 and hh % 2 == 0), stop=(mb == 1), skip_group_check=True),
                             reads=["PTm" + sx, "Vm"], writes=pk(bank))
                for half, bank in ((0, B3), (1, B0)):
                    pv_ = PS[:, bank, 0:260].rearrange("p (s e) -> p s e", e=130)
                    S.op("dve", lambda e: e.reciprocal(out=rcm[p][:, 2 * half:2 * half + 2], in_=pv_[:, :, 128]),
                         reads=pk(bank), writes=["rcm%d" % half + sx])
                    S.op("dve", lambda e: e.tensor_tensor(out=om[p][:, half * 256:(half + 1) * 256].rearrange("p (s d) -> p s d", s=2),
                                                          in0=pv_[:, :, 0:128],
                                                          in1=rcm[p][:, 2 * half:2 * half + 2].unsqueeze(2).to_broadcast([P, 2, 128]),
                                                          op=ALU.mult),
                         reads=pk(bank) + ["rcm%d" % half + sx], writes=["om" + sx])
            A(pv)
            A(lambda: transpose_to(om[p], "om" + sx, 4, B1, omT[p][:].rearrange("p c t -> p (c t)"), "omT" + sx))

            def oproj():
                for hf in range(2):
                    for c in range(4):
                        S.op("pe", lambda e: e.matmul(PS[:, B2 + hf, :], lhsT=omT[p][:, c, :], rhs=Wmo[:, c, hf * 512:(hf + 1) * 512],
                                                      start=(c == 0), stop=(c == 3)), reads=["omT" + sx, "Wmo"], writes=pk(B2 + hf))
                S.op("dve", lambda e: e.tensor_tensor(out=x2[p][:], in0=PS[:, B2:B2 + 2, :].rearrange("p a b -> p (a b)"), in1=x1[p][:], op=ALU.add),
                     reads=pk(B2, B3) + ["x1" + sx], writes=["x2" + sx])
                S.dma("sp", lambda e: e.dma_start(out=x2_d[t], in_=x2[p][:]), "x2st", reads=["x2" + sx], writes=["x2_d%d" % t])
                if debug:
                    S.dma("sp", lambda e: e.dma_start(out=dbg["x2"][t], in_=x2[p][:]), "dbg", reads=["x2" + sx])
            A(oproj)
            return st

        Acur = c1_steps(0)
        hsplit = 7
        for f_ in Acur[:hsplit]:
            f_()
        for t in range(NT):
            Bn = c1_steps(t + 1) if t + 1 < NT else []
            i, j = hsplit, 0
            jmax = min(hsplit, len(Bn))
            while i < len(Acur) or j < jmax:
                if i < len(Acur):
                    Acur[i]()
                    i += 1
                if j < jmax:
                    Bn[j]()
                    j += 1
            Acur = Bn
        S.barrier()

    if stop_after is None:
      with ExitStack() as es:
        def TT(name, shape, dt):
            return es.enter_context(nc.sbuf_tensor(name, shape, dt))
        Wpq = TT("Wpq", [P, KC, D], BF16)
        skT = TT("skT", [P, 8, 128], BF16)
        gffn = TT("gffn", [P, D], F32)
        gfin = TT("gfin", [P, D], F32)
        iota16 = TT("iota16", [P, 16], F32)
        x2 = [TT("x2P%d" % i, [P, D], F32) for i in range(2)]
        junk = TT("junkP", [P, D], BF16)
        junk2 = TT("junkP2", [P, D], BF16)
        junk3 = TT("junkP3", [P, D], BF16)
        DVEACC = 1000000
        ssq = TT("ssqP", [P, 1], F32)
        rstd = TT("rstdP", [P, 1], F32)
        ssq2 = TT("ssqP2", [P, 1], F32)
        rstd2 = TT("rstdP2", [P, 1], F32)
        h3 = TT("h3", [P, D], F32)
        h3b = [TT("h3b%d" % i, [P, D], BF16) for i in range(2)]
        h3T = TT("h3T", [P, KC, 128], BF16)
        pqT = TT("pqT", [P, 8, 128], BF16)
        scs = TT("scs", [P, 2, 8, 128], F32)
        scw = TT("scw", [P, 2, 8, 128], F32)
        tv = TT("tv", [P, 2, 8, 16], F32)
        tiu = TT("tiu", [P, 2, 8, 16], U32)
        tif = TT("tif", [P, 2, 8, 16], F32)
        cand = TT("cand", [P, 8, 16, 16], F32)
        candw = TT("candw", [P, 8, 16, 16], F32)
        bs = TT("bs", [P, 8, 16], F32)
        bpu = TT("bpu", [P, 8, 16], U32)
        bpi = TT("bpi", [P, 8, 16], U32)
        bpj = TT("bpj", [P, 8, 16], U32)
        bif = TT("bif", [P, 8, 16], F32)
        bjf = TT("bjf", [P, 8, 16], F32)
        eq = TT("eq", [P, 128, 16], F32)
        k1f = TT("k1f", [P, 128], F32)
        k2f = TT("k2f", [P, 128], F32)
        eidx = [TT("eidx%d" % i, [P, 128], I32) for i in range(2)]
        gate = [TT("gate%d" % i, [P, 8, 16], F32) for i in range(2)]
        gsum = TT("gsum", [P, 8], F32)
        aval = TT("aval", [P, 128], F32)
        gl = TT("gl", [P, 128], F32)
        NG = 16
        uv = [TT("uv%d" % i, [P, 2 * D], BF16) for i in range(NG)]
        NPR = 6
        prod = [TT("prod%d" % i, [P, D], BF16) for i in range(NPR)]
        dg = [TT("dg%d" % i, [P, P], BF16) for i in range(4)]
        x3 = TT("x3", [P, D], F32)
        ot = [TT("ot%d" % i, [P, D], F32) for i in range(2)]
        puv2 = puv_d.rearrange("e a d -> e (a d)")

        load_w_bf16(Wpq[:], wpq_d.rearrange("(c p) n -> p c n", p=P), "Wpq")
        load_w_bf16(skT[:], skt_d, "skT")
        S.dma("sp", lambda e: e.dma_start(out=gffn[:], in_=g_ffn_d.partition_broadcast(P)), "setup", writes=["gains"])
        S.dma("sp", lambda e: e.dma_start(out=gfin[:], in_=g_fin_d.partition_broadcast(P)), "setup", writes=["gainf"])
        S.op("pool", lambda e: e.iota(eidx[0][:, 0:16], pattern=[[1, 16]], base=0, channel_multiplier=0), writes=["eidx0"])
        S.op("dve", lambda e: e.tensor_copy(out=iota16[:], in_=eidx[0][:, 0:16]), reads=["eidx0"], writes=["iota16"])

        def topk_steps(t):
            b = t % 2
            st = []
            A = st.append
            A(lambda: S.dma("sp", lambda e: e.dma_start(out=x2[b][:], in_=x2_d[t]), "x2ld", writes=["x2%d" % b]))
            A(lambda: rmsnorm_tile(x2[b][:], "x2%d" % b, gffn, h3[:], "h3", "P", junk, ssq, rstd))
            A(lambda: S.op("act", lambda e: e.copy(out=h3b[b][:], in_=h3[:]), reads=["h3"], writes=["h3b%d" % b]))
            A(lambda: transpose_to(h3b[b], "h3b%d" % b, KC, 0, h3T[:].rearrange("p c t -> p (c t)"), "h3T"))

            def qT(hh):
                for c in range(KC):
                    S.op("pe", lambda e: e.matmul(PS[:, 1 + hh // 4, (hh % 4) * 128:(hh % 4 + 1) * 128],
                                                  lhsT=Wpq[:, c, hh * 128:(hh + 1) * 128], rhs=h3T[:, c, :],
                                                  start=(c == 0 and hh % 4 == 0), stop=(c == KC - 1), skip_group_check=True),
                         reads=["Wpq", "h3T"], writes=pk(1 + hh // 4))
            for hh in range(8):
                A(lambda hh=hh: qT(hh))
            A(lambda: S.op("act", lambda e: e.copy(out=pqT[:].rearrange("p h t -> p (h t)"), in_=PS[:, 1:3, :].rearrange("p a b -> p (a b)")),
                           reads=pk(1, 2), writes=["pqT"]))

            def sc(c2):
                for hh in range(8):
                    bank = (0 if c2 == 0 else 2) + hh // 4
                    S.op("pe", lambda e: e.matmul(PS[:, bank, (hh % 4) * 128:(hh % 4 + 1) * 128],
                                                  lhsT=pqT[64 * c2:64 * c2 + 64, hh, :], rhs=skT[64 * c2:64 * c2 + 64, hh, :],
                                                  start=(hh % 4 == 0), stop=True, skip_group_check=True),
                         reads=["pqT", "skT"], writes=pk(bank))
                bk = 0 if c2 == 0 else 2
                S.op("act", lambda e: e.copy(out=scs[:, c2, :, :].rearrange("p h k -> p (h k)"),
                                             in_=PS[:, bk:bk + 2, :].rearrange("p a b -> p (a b)")),
                     reads=pk(bk, bk + 1), writes=["scs%d" % c2])
            A(lambda: sc(0))
            A(lambda: sc(1))

            grp = [(c2, hh) for c2 in range(2) for hh in range(8)]

            def t16(stage, gs):
                for (c2, hh) in gs:
                    g = c2 * 8 + hh
                    sk = "scs%d" % c2
                    if stage == 0:
                        S.op("dve", lambda e: e.max(out=tv[:, c2, hh, 0:8], in_=scs[:, c2, hh, :]), reads=[sk], writes=["tva%d" % g])
                    elif stage == 1:
                        S.op("dve", lambda e: e.max_index(out=tiu[:, c2, hh, 0:8], in_max=tv[:, c2, hh, 0:8], in_values=scs[:, c2, hh, :]),
                             reads=[sk, "tva%d" % g], writes=["tiua%d" % g])
                    elif stage == 2:
                        S.op("dve", lambda e: e.match_replace(out=scw[:, c2, hh, :], in_to_replace=tv[:, c2, hh, 0:8],
                                                              in_values=scs[:, c2, hh, :], imm_value=NEG),
                             reads=[sk, "tva%d" % g], writes=["scw%d" % g])
                    elif stage == 3:
                        S.op("dve", lambda e: e.max(out=tv[:, c2, hh, 8:16], in_=scw[:, c2, hh, :]), reads=["scw%d" % g], writes=["tvb%d" % g])
                    else:
                        S.op("dve", lambda e: e.max_index(out=tiu[:, c2, hh, 8:16], in_max=tv[:, c2, hh, 8:16], in_values=scw[:, c2, hh, :]),
                             reads=["scw%d" % g, "tvb%d" % g], writes=["tiub%d" % g])
            for stage in range(5):
                for half in range(2):
                    A(lambda stage=stage, half=half: t16(stage, grp[half * 8:(half + 1) * 8]))
            tvk = ["tva%d" % g for g in range(16)] + ["tvb%d" % g for g in range(16)]
            tik = ["tiua%d" % g for g in range(16)] + ["tiub%d" % g for g in range(16)]
            A(lambda: S.op("dve", lambda e: e.tensor_copy(out=tif[:], in_=tiu[:]), reads=tik, writes=["tif"]))
            A(lambda: S.op("dve", lambda e: e.tensor_tensor(out=cand[:], in0=tv[:, 0, :, :].unsqueeze(3).to_broadcast([P, 8, 16, 16]),
                                                            in1=tv[:, 1, :, :].unsqueeze(2).to_broadcast([P, 8, 16, 16]), op=ALU.add),
                           reads=tvk, writes=["cand"]))

            def b16(stage):
                for hh in range(8):
                    cv = cand[:, hh, :, :].rearrange("p i j -> p (i j)")
                    cw = candw[:, hh, :, :].rearrange("p i j -> p (i j)")
                    if stage == 0:
                        S.op("dve", lambda e: e.max(out=bs[:, hh, 0:8], in_=cv), reads=["cand"], writes=["bsa%d" % hh])
                    elif stage == 1:
                        S.op("dve", lambda e: e.max_index(out=bpu[:, hh, 0:8], in_max=bs[:, hh, 0:8], in_values=cv),
                             reads=["cand", "bsa%d" % hh], writes=["bpua%d" % hh])
                    elif stage == 2:
                        S.op("dve", lambda e: e.match_replace(out=cw, in_to_replace=bs[:, hh, 0:8], in_values=cv, imm_value=NEG),
                             reads=["cand", "bsa%d" % hh], writes=["candw%d" % hh])
                    elif stage == 3:
                        S.op("dve", lambda e: e.max(out=bs[:, hh, 8:16], in_=cw), reads=["candw%d" % hh], writes=["bsb%d" % hh])
                    else:
                        S.op("dve", lambda e: e.max_index(out=bpu[:, hh, 8:16], in_max=bs[:, hh, 8:16], in_values=cw),
                             reads=["candw%d" % hh, "bsb%d" % hh], writes=["bpub%d" % hh])
            for stage in range(5):
                A(lambda stage=stage: b16(stage))
            bsk = ["bsa%d" % h for h in range(8)] + ["bsb%d" % h for h in range(8)]
            bpk = ["bpua%d" % h for h in range(8)] + ["bpub%d" % h for h in range(8)]

            def posij():
                S.op("dve", lambda e: e.tensor_single_scalar(out=bpi[:], in_=bpu[:], scalar=4, op=ALU.logical_shift_right),
                     reads=bpk, writes=["bpi"])
                S.op("dve", lambda e: e.tensor_single_scalar(out=bpj[:], in_=bpu[:], scalar=15, op=ALU.bitwise_and),
                     reads=bpk, writes=["bpj"])
                S.op("dve", lambda e: e.tensor_copy(out=bif[:], in_=bpi[:]), reads=["bpi"], writes=["bif"])
                S.op("dve", lambda e: e.tensor_copy(out=bjf[:], in_=bpj[:]), reads=["bpj"], writes=["bjf"])
            A(posij)

            def kx(pf_, c2, kf):
                S.op("dve", lambda e: e.tensor_tensor(out=eq[:], in0=iota16[:].unsqueeze(1).to_broadcast([P, 128, 16]),
                                                      in1=pf_[:].rearrange("p h r -> p (h r)").unsqueeze(2).to_broadcast([P, 128, 16]),
                                                      op=ALU.is_equal),
                     reads=["iota16", "bif", "bjf"], writes=["eq"])
                S.op("dve", lambda e: e.tensor_tensor(out=eq[:].rearrange("p (h r) i -> p h r i", h=8),
                                                      in0=eq[:].rearrange("p (h r) i -> p h r i", h=8),
                                                      in1=tif[:, c2, :, :].unsqueeze(2).to_broadcast([P, 8, 16, 16]), op=ALU.mult),
                     reads=["eq", "tif"], writes=["eq"])
                S.op("dve", lambda e: e.tensor_reduce(out=kf[:], in_=eq[:], axis=AX.X, op=ALU.add), reads=["eq"], writes=["kf%d" % c2])
            A(lambda: kx(bif, 0, k1f))
            A(lambda: kx(bjf, 1, k2f))

            def fin():
                S.op("dve", lambda e: e.scalar_tensor_tensor(out=k1f[:], in0=k1f[:], scalar=128.0, in1=k2f[:], op0=ALU.mult, op1=ALU.add),
                     reads=["kf0", "kf1"], writes=["kf0"])
                S.op("dve", lambda e: e.tensor_copy(out=eidx[b][:], in_=k1f[:]), reads=["kf0"], writes=["eidx%d" % b])
                S.op("dve", lambda e: e.tensor_tensor(out=gate[b][:], in0=bs[:], in1=bs[:, :, 0:1].to_broadcast([P, 8, 16]), op=ALU.subtract),
                     reads=bsk, writes=["gate%d" % b])
                S.op("act", lambda e: e.activation(out=gate[b][:], in_=gate[b][:], func=AF.Exp), reads=["gate%d" % b], writes=["gate%d" % b])
                S.op("dve", lambda e: e.tensor_reduce(out=gsum[:], in_=gate[b][:], axis=AX.X, op=ALU.add), reads=["gate%d" % b], writes=["gsum"])
                S.op("dve", lambda e: e.reciprocal(out=gsum[:], in_=gsum[:]), reads=["gsum"], writes=["gsum"])
                S.op("dve", lambda e: e.tensor_tensor(out=gate[b][:], in0=gate[b][:], in1=gsum[:].unsqueeze(2).to_broadcast([P, 8, 16]), op=ALU.mult),
                     reads=["gate%d" % b, "gsum"], writes=["gate%d" % b])
            A(fin)
            return st

        gcount = [0]

        def slot(t, sl):
            b = t % 2
            gb = gcount[0] % NG
            gcount[0] += 1
            pb_ = sl % NPR
            db = sl % 4
            S.dma("pool", lambda e: e.indirect_dma_start(out=uv[gb][:], out_offset=None, in_=puv2,
                                                         in_offset=bass.IndirectOffsetOnAxis(ap=eidx[b][:, sl:sl + 1], axis=0)),
                  "uv%d" % gb, reads=["eidx%d" % b], writes=["uv%d" % gb])
            S.op("dve", lambda e: e.tensor_tensor(out=prod[pb_][:], in0=uv[gb][:, 0:D], in1=h3b[b][:], op=ALU.mult),
                 reads=["uv%d" % gb, "h3b%d" % b], writes=["prod%d" % pb_])
            ag = (sl // 4) % 4
            if sl % DVEACC == DVEACC - 1:
                S.op("dve", lambda e: e.tensor_scalar(out=junk3[:], in0=prod[pb_][:], scalar1=1.0, scalar2=None, op0=ALU.mult, op1=ALU.add,
                                                      accum_out=aval[:, sl:sl + 1]),
                     reads=["prod%d" % pb_], writes=["aval%d_%d" % (ag, sl % 4)])
            else:
                S.op("act", lambda e: e.activation(out=junk2[:], in_=prod[pb_][:], func=AF.Copy, accum_out=aval[:, sl:sl + 1]),
                     reads=["prod%d" % pb_], writes=["aval%d_%d" % (ag, sl % 4)])
            if sl % 4 == 3:
                S.op("act", lambda e: e.activation(out=gl[:, sl - 3:sl + 1], in_=aval[:, sl - 3:sl + 1], func=AF.Gelu),
                     reads=["aval%d_%d" % (ag, i) for i in range(4)], writes=["gl%d" % ag])
            pend.append((t, sl, gb))
            if len(pend) > LAG:
                second(*pend.pop(0))

        LAG = 6
        pend = []

        def second(t, sl, gb):
            b = t % 2
            db = sl % 4
            ag = (sl // 4) % 4
            gfl = gate[b][:].rearrange("p h r -> p (h r)")
            S.op("dve", lambda e: e.tensor_scalar(out=dg[db][:], in0=ident[:], scalar1=gl[:, sl:sl + 1], scalar2=gfl[:, sl:sl + 1],
                                                  op0=ALU.mult, op1=ALU.mult),
                 reads=["ident", "gl%d" % ag, "gate%d" % b], writes=["dg%d" % db])
            ab = 4 + 2 * b
            for hf in range(2):
                S.op("pe", lambda e: e.matmul(PS[:, ab + hf, :], lhsT=dg[db][:], rhs=uv[gb][:, D + hf * 512:D + (hf + 1) * 512],
                                              start=(sl == 0), stop=(sl == 127)),
                     reads=["dg%d" % db, "uv%d" % gb], writes=pk(ab + hf))
            if sl == 127:
                finalize(t)

        def finalize(t):
            b = t % 2
            ab = 4 + 2 * b
            S.op("dve", lambda e: e.tensor_tensor(out=x3[:], in0=PS[:, ab:ab + 2, :].rearrange("p a b -> p (a b)"), in1=x2[b][:], op=ALU.add),
                 reads=pk(ab, ab + 1) + ["x2%d" % b], writes=["x3"])
            rmsnorm_tile(x3[:], "x3", gfin, ot[b][:], "ot%d" % b, "P2", junk2, ssq2, rstd2, gkey="gainf")
            S.dma("sp", lambda e: e.dma_start(out=outT[t], in_=ot[b][:]), "out", reads=["ot%d" % b])

        ntc = NT
        for f_ in topk_steps(0):
            f_()
        for t in range(ntc):
            nxt = topk_steps(t + 1) if t + 1 < ntc else []
            ni = 0
            for sl in range(128):
                slot(t, sl)
                if sl >= 16 and ni < len(nxt):
                    want = ((sl - 15) * len(nxt) + 103) // 104
                    while ni < min(want, len(nxt)):
                        nxt[ni]()
                        ni += 1
            while ni < len(nxt):
                nxt[ni]()
                ni += 1
        while pend:
            second(*pend.pop(0))
        S.barrier()

    S.barrier()
    gstack.close()
    return nc, S


_CACHE = {}


def kernel(x, mem, positions, norm_mix_g, w_in, da_lambda, da_subln_g, w_branch_a, w_branch_b, gate_bias, w_out,
           norm_mem_g, mem_kv_norm_g, w_mem_q, w_mem_kv, w_mem_o, norm_ffn_g, peer_w_q, peer_sub_keys, peer_u, peer_v,
           final_norm_g):
    n = 8
    f = lambda a: np.ascontiguousarray(np.asarray(a))
    if "nc" not in _CACHE:
        _CACHE["nc"] = build_program()[0]
    nc = _CACHE["nc"]
    skt = f(np.asarray(peer_sub_keys)[0].transpose(1, 3, 0, 2).reshape(128, 8, 128))
    shared = {
        "norm_mix_g": f(norm_mix_g[0]), "w_in": f(w_in[0]), "da_lambda": f(np.asarray(da_lambda)[0].reshape(256)),
        "da_subln_g": f(da_subln_g[0]), "w_branch_a": f(w_branch_a[0]), "w_branch_b": f(w_branch_b[0]),
        "gate_bias": f(gate_bias[0]), "w_out": f(w_out[0]), "norm_mem_g": f(norm_mem_g[0]),
        "mem_kv_norm_g": f(mem_kv_norm_g[0]), "w_mem_q": f(w_mem_q[0]), "w_mem_kv": f(w_mem_kv[0]),
        "w_mem_o": f(w_mem_o[0]), "norm_ffn_g": f(norm_ffn_g[0]), "peer_w_q": f(peer_w_q[0]), "peer_skt": skt,
        "peer_u": f(peer_u[0]), "peer_v": f(peer_v[0]), "final_norm_g": f(final_norm_g),
    }
    in_maps = []
    for b in range(n):
        m = dict(shared)
        m["x"] = f(x[b])
        m["mem"] = f(mem[b])
        m["pos"] = f(np.asarray(positions)[b].astype(np.int32).reshape(NT, P).T)
        in_maps.append(m)
    res = run_bass_kernel_spmd(nc, in_maps, core_ids=list(range(n)))
    return np.stack([np.asarray(r["out"]) for r in res.results], axis=0).astype(np.float32)
```

```python
import math
import os
from contextlib import ExitStack

import numpy as np
import concourse.bass as bass
import concourse.mybir as mybir
from concourse.bass_utils import run_bass_kernel_spmd

F32 = mybir.dt.float32
BF16 = mybir.dt.bfloat16
I32 = mybir.dt.int32
U32 = mybir.dt.uint32
ALU = mybir.AluOpType
AF = mybir.ActivationFunctionType
AX = mybir.AxisListType

EPOCH = 30000
P = 128
SL = 4096
NT = 32
D = 1024
KC = 8
EPS = 1e-6
NEG = -1.0e30
NSEL = 256
NBIS = 16
PI = float(np.pi)


class Sched:
    def __init__(self, nc, same_engine_sync=True):
        self.nc = nc
        self.engs = {"pe": nc.tensor, "dve": nc.vector, "act": nc.scalar,
                     "pool": nc.gpsimd, "sp": nc.sync}
        self.sem = {}
        self.cnt = {}
        self.nsem = 0
        self.waited = {e: {} for e in self.engs}
        self.same = same_engine_sync
        self.res = {}
        self.dma_sem = {}
        self.dma_total = {}
        self.all_dma = []
        self.ninst = {e: 0 for e in self.engs}

    def _newsem(self, name):
        self.nsem += 1
        return self.nc.alloc_semaphore(f"{name}_{self.nsem}")

    def _eng_token(self, e):
        if e not in self.sem or self.cnt[e] >= EPOCH:
            self.sem[e] = self._newsem(f"s_{e}")
            self.cnt[e] = 0
        self.cnt[e] += 1
        return (self.sem[e], self.cnt[e], e)

    def _wait(self, e, tok):
        sem, val, src = tok
        if src == "dma":
            val = self.dma_total[sem.name]
        elif src == e and (e == "pe" or not self.same):
            return
        w = self.waited[e]
        if w.get(sem.name, 0) >= val:
            return
        self.engs[e].wait_ge(sem, val)
        w[sem.name] = val

    def _deps(self, e, reads, writes):
        for k in reads:
            r = self.res.get(k)
            if r is not None:
                for t in r[0]:
                    self._wait(e, t)
        for k in writes:
            r = self.res.get(k)
            if r is not None:
                for t in r[0]:
                    self._wait(e, t)
                for t in r[1]:
                    self._wait(e, t)

    def _record(self, tok, reads, writes):
        for k in reads:
            r = self.res.setdefault(k, [[], []])
            r[1].append(tok)
            if len(r[1]) > 48:
                r[1] = self._compact(r[1])
        for k in writes:
            self.res[k] = [[tok], []]

    @staticmethod
    def _compact(toks):
        best = {}
        for t in toks:
            key = (t[0].name, t[2])
            if key not in best or best[key][1] < t[1]:
                best[key] = t
        return list(best.values())

    def op(self, e, fn, reads=(), writes=()):
        psr = [k for k in reads if k.startswith("ps")]
        if psr:
            reads = [k for k in reads if not k.startswith("ps")]
            writes = list(writes) + [k for k in psr if k not in writes]
        self._deps(e, reads, writes)
        ins = fn(self.engs[e])
        tok = self._eng_token(e)
        ins.then_inc(tok[0], 1)
        self._record(tok, reads, writes)
        self.ninst[e] += 1
        return tok

    def dma(self, q, fn, semkey, reads=(), writes=()):
        self._deps(q, reads, writes)
        if semkey not in self.dma_sem or self.dma_total[self.dma_sem[semkey].name] + 16 > EPOCH:
            s = self._newsem("d")
            self.dma_sem[semkey] = s
            self.dma_total[s.name] = 0
            self.all_dma.append(s)
        s = self.dma_sem[semkey]
        ins = fn(self.engs[q])
        self.dma_total[s.name] += 16
        ins.then_inc(s, 16)
        tok = (s, self.dma_total[s.name], "dma")
        self._record(tok, reads, writes)
        self.ninst[q] += 1
        return tok

    def barrier(self):
        toks = []
        for e in ("pe", "dve", "act", "pool"):
            if e in self.sem:
                toks.append((self.sem[e], self.cnt[e], e))
        for e in self.engs:
            for t in toks:
                if t[2] != e:
                    self._wait(e, t)
            for s in self.all_dma:
                if self.dma_total[s.name] > 0:
                    self._wait(e, (s, self.dma_total[s.name], "dma"))
        self.res = {}


C_DAQ, C_DAK, C_DAV, C_DSQ, C_DSK, C_DSV, C_IXQ, C_IXK, C_IXW, C_GATE = (
    0, 512, 1024, 1536, 2048, 2176, 2304, 2816, 2880, 2888)


def build_program(debug=False, stop_after=None):
    nc = bass.Bass("TRN2", target_bir_lowering=False)
    S = Sched(nc, same_engine_sync=True)

    def din(name, shape, dt=F32):
        return nc.dram_tensor(name, shape, dt, kind="ExternalInput").ap()

    x_d = din("x", [SL, D])
    mem_d = din("mem", [256, D])
    pos_d = din("pos", [P, NT], I32)
    g_mix_d = din("norm_mix_g", [D])
    w_in_d = din("w_in", [D, 4936])
    lam_d = din("da_lambda", [256])
    subg_d = din("da_subln_g", [128])
    wa_d = din("w_branch_a", [512, D])
    wb_d = din("w_branch_b", [512, D])
    gbias_d = din("gate_bias", [2048])
    wo_d = din("w_out", [D, D])
    g_mem_d = din("norm_mem_g", [D])
    g_kv_d = din("mem_kv_norm_g", [D])
    wmq_d = din("w_mem_q", [D, 512])
    wmkv_d = din("w_mem_kv", [D, D])
    wmo_d = din("w_mem_o", [512, D])
    g_ffn_d = din("norm_ffn_g", [D])
    wpq_d = din("peer_w_q", [D, D])
    skt_d = din("peer_skt", [P, 8, 128])
    pu_d = din("peer_u", [16384, D])
    pv_d = din("peer_v", [16384, D])
    g_fin_d = din("final_norm_g", [D])
    out_d = nc.dram_tensor("out", [SL, D], F32, kind="ExternalOutput").ap()

    hnT_d = nc.dram_tensor("hnT_scr", [NT, P, KC * 128], BF16).ap()
    oaT_d = nc.dram_tensor("oaT_scr", [NT, P, 4 * 128], BF16).ap()
    obT_d = nc.dram_tensor("obT_scr", [NT, P, 4 * 128], BF16).ap()
    x2_d = nc.dram_tensor("x2_scr", [NT, P, D], F32).ap()
    puv_d = nc.dram_tensor("puv_scr", [16384, 2, D], BF16).ap()

    dbg = {}
    if debug:
        dbg["oa"] = nc.dram_tensor("dbg_oa", [NT, P, 512], BF16, kind="ExternalOutput").ap()
        dbg["ob"] = nc.dram_tensor("dbg_ob", [NT, P, 512], BF16, kind="ExternalOutput").ap()
        dbg["kt"] = nc.dram_tensor("dbg_kt", [P, 4 * SL], BF16, kind="ExternalOutput").ap()
        dbg["x2"] = nc.dram_tensor("dbg_x2", [NT, P, D], F32, kind="ExternalOutput").ap()
        dbg["x1"] = nc.dram_tensor("dbg_x1", [NT, P, D], F32, kind="ExternalOutput").ap()

    xT = x_d.rearrange("(t p) d -> t p d", p=P)
    outT = out_d.rearrange("(t p) d -> t p d", p=P)

    PS = nc.alloc_psum_tensor("PS", [P, 8, 512], F32)

    def psbf(b):
        return PS[:, b, :].bitcast(BF16)

    def pk(*bs):
        return ["ps%d" % b for b in bs]

    gstack = ExitStack()

    def GT(name, shape, dt):
        return gstack.enter_context(nc.sbuf_tensor(name, shape, dt))

    iot = GT("iot", [P, P], I32)
    ident = GT("ident", [P, P], BF16)
    tri01 = GT("tri01", [P, P], BF16)
    triNEG = GT("triNEG", [P, P], F32)
    CC = GT("CC", [P, NT, 16], F32)
    SSt = GT("SSt", [P, NT, 16], F32)
    neglam = GT("neglam", [P, 1], F32)
    subg08 = GT("subg08", [P, 128], F32)

    S.op("pool", lambda e: e.iota(iot[:], pattern=[[1, P]], base=0, channel_multiplier=-1), writes=["iot"])
    S.op("dve", lambda e: e.tensor_scalar(out=ident[:], in0=iot[:], scalar1=0.0, scalar2=None, op0=ALU.is_equal),
         reads=["iot"], writes=["ident"])
    S.op("dve", lambda e: e.tensor_scalar(out=tri01[:], in0=iot[:], scalar1=0.0, scalar2=None, op0=ALU.is_ge),
         reads=["iot"], writes=["tri01"])
    S.op("dve", lambda e: e.tensor_scalar(out=triNEG[:], in0=iot[:], scalar1=0.0, scalar2=NEG, op0=ALU.is_gt, op1=ALU.mult),
         reads=["iot"], writes=["triNEG"])

    with ExitStack() as es:
        def TT(name, shape, dt):
            return es.enter_context(nc.sbuf_tensor(name, shape, dt))
        posi = TT("posi", [P, NT], I32)
        posf = TT("posf", [P, NT], F32)
        invf = TT("invf", [P, 8], F32)
        ang = TT("ang", [P, NT, 8], F32)
        ang2 = TT("ang2", [P, NT, 8], F32)
        ki_ = TT("ki_", [P, NT, 8], I32)
        kf_ = TT("kf_", [P, NT, 8], F32)
        m1_ = TT("m1_", [P, NT, 8], F32)
        lamt = TT("lamt", [P, 256], F32)
        lj = TT("lj", [P, 64], F32)
        s01 = TT("s01", [P, 2], F32)

        S.dma("sp", lambda e: e.dma_start(out=posi[:], in_=pos_d), "setup", writes=["posi"])
        S.dma("sp", lambda e: e.dma_start(out=lamt[:], in_=lam_d.partition_broadcast(P)), "setup", writes=["lamt"])
        S.dma("sp", lambda e: e.dma_start(out=subg08[:], in_=subg_d.partition_broadcast(P)), "setup", writes=["subg08"])
        S.op("dve", lambda e: e.tensor_copy(out=posf[:], in_=posi[:]), reads=["posi"], writes=["posf"])
        inv = (np.float32(500000.0) ** (-(np.arange(8, dtype=np.float32) * np.float32(2.0)) / np.float32(16.0))).astype(np.float32)
        for i in range(8):
            S.op("dve", lambda e: e.memset(invf[:, i:i + 1], float(inv[i])), writes=["invf"])
        S.op("dve", lambda e: e.tensor_tensor(out=ang[:], in0=posf[:].unsqueeze(2).to_broadcast([P, NT, 8]),
                                              in1=invf[:].unsqueeze(1).to_broadcast([P, NT, 8]), op=ALU.mult),
             reads=["posf", "invf"], writes=["ang"])
        S.op("dve", lambda e: e.tensor_scalar(out=ang2[:], in0=ang[:], scalar1=PI / 2, scalar2=None, op0=ALU.add),
             reads=["ang"], writes=["ang2"])
        C1 = 6.28125
        C2 = float(2 * np.pi - 6.28125)

        def sin_of(a, key):
            S.op("dve", lambda e: e.tensor_scalar(out=ki_[:], in0=a[:], scalar1=float(1 / (2 * np.pi)), scalar2=None, op0=ALU.mult),
                 reads=[key], writes=["ki_"])
            S.op("dve", lambda e: e.tensor_copy(out=kf_[:], in_=ki_[:]), reads=["ki_"], writes=["kf_"])
            S.op("dve", lambda e: e.scalar_tensor_tensor(out=a[:], in0=kf_[:], scalar=-C1, in1=a[:], op0=ALU.mult, op1=ALU.add),
                 reads=["kf_", key], writes=[key])
            S.op("dve", lambda e: e.scalar_tensor_tensor(out=a[:], in0=kf_[:], scalar=-C2, in1=a[:], op0=ALU.mult, op1=ALU.add),
                 reads=["kf_", key], writes=[key])
            S.op("dve", lambda e: e.tensor_scalar(out=m1_[:], in0=a[:], scalar1=PI, scalar2=-2 * PI, op0=ALU.is_gt, op1=ALU.mult),
                 reads=[key], writes=["m1_"])
            S.op("dve", lambda e: e.tensor_tensor(out=a[:], in0=a[:], in1=m1_[:], op=ALU.add), reads=[key, "m1_"], writes=[key])
            S.op("dve", lambda e: e.tensor_scalar(out=m1_[:], in0=a[:], scalar1=-PI, scalar2=2 * PI, op0=ALU.is_lt, op1=ALU.mult),
                 reads=[key], writes=["m1_"])
            S.op("dve", lambda e: e.tensor_tensor(out=a[:], in0=a[:], in1=m1_[:], op=ALU.add), reads=[key, "m1_"], writes=[key])
            S.op("act", lambda e: e.activation(out=a[:], in_=a[:], func=AF.Sin), reads=[key], writes=[key])

        sin_of(ang, "ang")
        sin_of(ang2, "ang2")
        S.op("dve", lambda e: e.tensor_copy(out=CC[:, :, 0:8], in_=ang2[:]), reads=["ang2"], writes=["CC"])
        S.op("dve", lambda e: e.tensor_copy(out=CC[:, :, 8:16], in_=ang2[:]), reads=["ang2"], writes=["CC"])
        S.op("dve", lambda e: e.tensor_scalar(out=SSt[:, :, 0:8], in0=ang[:], scalar1=-1.0, scalar2=None, op0=ALU.mult),
             reads=["ang"], writes=["SSt"])
        S.op("dve", lambda e: e.tensor_copy(out=SSt[:, :, 8:16], in_=ang[:]), reads=["ang"], writes=["SSt"])
        S.op("dve", lambda e: e.scalar_tensor_tensor(out=lj[:], in0=lamt[:, 0:64], scalar=1.0, in1=lamt[:, 64:128],
                                                     op0=ALU.mult, op1=ALU.mult, accum_out=s01[:, 0:1]),
             reads=["lamt"], writes=["lj", "s01"])
        S.op("dve", lambda e: e.scalar_tensor_tensor(out=lj[:], in0=lamt[:, 128:192], scalar=1.0, in1=lamt[:, 192:256],
                                                     op0=ALU.mult, op1=ALU.mult, accum_out=s01[:, 1:2]),
             reads=["lamt", "lj", "s01"], writes=["lj", "s01"])
        S.op("act", lambda e: e.activation(out=s01[:], in_=s01[:], func=AF.Exp), reads=["s01"], writes=["s01"])
        S.op("dve", lambda e: e.tensor_tensor(out=neglam[:], in0=s01[:, 1:2], in1=s01[:, 0:1], op=ALU.subtract),
             reads=["s01"], writes=["neglam"])
        S.op("dve", lambda e: e.tensor_scalar(out=neglam[:], in0=neglam[:], scalar1=-0.2, scalar2=None, op0=ALU.add),
             reads=["neglam"], writes=["neglam"])
        S.op("dve", lambda e: e.tensor_scalar(out=subg08[:], in0=subg08[:], scalar1=0.8, scalar2=None, op0=ALU.mult),
             reads=["subg08"], writes=["subg08"])
        S.barrier()

    def load_w_bf16(dst_ap, src_ap, key):
        S.dma("pool", lambda e: e.dma_start(out=dst_ap, in_=src_ap), "wload", writes=[key])

    def rmsnorm_tile(xt_ap, xkey, gain, hn_ap, hnkey, sfx, junk, ssq, rstd, extra_w=(), gkey="gains"):
        S.op("act", lambda e: e.activation(out=junk[:], in_=xt_ap, func=AF.Square, accum_out=ssq[:]),
             reads=[xkey], writes=["junk" + sfx, "ssq" + sfx])
        S.op("act", lambda e: e.activation(out=rstd[:], in_=ssq[:], func=AF.Sqrt, scale=1.0 / D, bias=EPS),
             reads=["ssq" + sfx], writes=["rstd" + sfx])
        S.op("dve", lambda e: e.reciprocal(out=rstd[:], in_=rstd[:]), reads=["rstd" + sfx], writes=["rstd" + sfx])
        S.op("dve", lambda e: e.scalar_tensor_tensor(out=hn_ap, in0=xt_ap, scalar=rstd[:], in1=gain[:],
                                                     op0=ALU.mult, op1=ALU.mult),
             reads=[xkey, "rstd" + sfx, gkey], writes=[hnkey] + list(extra_w))

    def transpose_to(src_tile, skey, nchunk, bank, dst_ap, dkey, eng="act"):
        for c in range(nchunk):
            S.op("pe", lambda e: e.transpose(out=psbf(bank)[:, c * 128:(c + 1) * 128],
                                             in_=src_tile[:, c * 128:(c + 1) * 128], identity=ident[:]),
                 reads=[skey, "ident"], writes=pk(bank))
        if eng == "act":
            S.op("act", lambda e: e.copy(out=dst_ap, in_=psbf(bank)[:, 0:nchunk * 128]), reads=pk(bank), writes=[dkey])
        else:
            S.op("dve", lambda e: e.tensor_copy(out=dst_ap, in_=psbf(bank)[:, 0:nchunk * 128]), reads=pk(bank), writes=[dkey])

    def proj(lhsT_tile, lkey, nk, W, wkey, c0, n, bank, boff=0):
        for c in range(nk):
            S.op("pe", lambda e: e.matmul(PS[:, bank, boff:boff + n], lhsT=lhsT_tile[:, c, :], rhs=W[:, c, c0:c0 + n],
                                          start=(c == 0), stop=(c == nk - 1)),
                 reads=[lkey, wkey], writes=pk(bank))

    def rope4(src3, dst3, nh, t, tA, tB, rkeys, dkey):
        cc = CC[:, t, :].unsqueeze(1).to_broadcast([P, nh, 16])
        s_lo = SSt[:, t, 0:8].unsqueeze(1).to_broadcast([P, nh, 8])
        s_hi = SSt[:, t, 8:16].unsqueeze(1).to_broadcast([P, nh, 8])
        S.op("act", lambda e: e.copy(out=ropeS[:, 0:nh, :], in_=src3[:, :, 0:16]), reads=rkeys, writes=["ropeS"])
        S.op("dve", lambda e: e.tensor_tensor(out=tA[:, 0:nh, :], in0=ropeS[:, 0:nh, :], in1=cc, op=ALU.mult),
             reads=["ropeS"], writes=["ropeA"])
        S.op("dve", lambda e: e.tensor_tensor(out=tB[:, 0:nh, 0:8], in0=ropeS[:, 0:nh, 8:16], in1=s_lo, op=ALU.mult),
             reads=["ropeS"], writes=["ropeB"])
        S.op("dve", lambda e: e.tensor_tensor(out=tB[:, 0:nh, 8:16], in0=ropeS[:, 0:nh, 0:8], in1=s_hi, op=ALU.mult),
             reads=["ropeS", "ropeB"], writes=["ropeB"])
        S.op("dve", lambda e: e.tensor_tensor(out=dst3[:, :, 0:16], in0=tA[:, 0:nh, :], in1=tB[:, 0:nh, :], op=ALU.add),
             reads=["ropeA", "ropeB", dkey], writes=[dkey])

    kv = ExitStack()

    def KVT(name, shape, dt):
        return kv.enter_context(nc.sbuf_tensor(name, shape, dt))

    KT_da = KVT("KT_da", [P, 4, SL], BF16)
    V_da = KVT("V_da", [P, NT, 4, 130], BF16)
    KT_ds = KVT("KT_ds", [P, SL], BF16)
    V_ds = KVT("V_ds", [P, NT, 2, 66], BF16)
    KT_ix = KVT("KT_ix", [P, SL], BF16)
    ropeA = KVT("ropeA", [P, 24, 16], F32)
    ropeB = KVT("ropeB", [P, 24, 16], F32)
    ropeS = KVT("ropeS", [P, 24, 16], F32)
    Wq = KVT("Wq", [P, KC, 1544], BF16)

    S.op("pool", lambda e: e.memset(V_da[:, :, :, 128:130], 1.0), writes=["Vda"])
    S.op("pool", lambda e: e.memset(V_ds[:, :, :, 64:66], 1.0), writes=["Vds"])

    with ExitStack() as es:
        def TT(name, shape, dt):
            return es.enter_context(nc.sbuf_tensor(name, shape, dt))
        Wkv = TT("Wkv", [P, KC, 1344], BF16)
        gmix = TT("gmix", [P, D], F32)
        xt = [TT("xtA%d" % i, [P, D], F32) for i in range(2)]
        junk = TT("junkA", [P, D], BF16)
        ssq = [TT("ssqA%d" % i, [P, 1], F32) for i in range(2)]
        rstd = [TT("rstdA%d" % i, [P, 1], F32) for i in range(2)]
        hn = [TT("hnA%d" % i, [P, D], BF16) for i in range(2)]
        hnT = [TT("hnTA%d" % i, [P, KC, 128], BF16) for i in range(2)]
        krAll = TT("krAll", [P, 704], BF16)

        w3 = w_in_d.rearrange("(c p) n -> p c n", p=P)
        load_w_bf16(Wkv[:, :, 0:512], w3[:, :, C_DAK:C_DAK + 512], "Wkv")
        load_w_bf16(Wkv[:, :, 512:640], w3[:, :, C_DSK:C_DSK + 128], "Wkv")
        load_w_bf16(Wkv[:, :, 640:704], w3[:, :, C_IXK:C_IXK + 64], "Wkv")
        load_w_bf16(Wkv[:, :, 704:832], w3[:, :, C_DSV:C_DSV + 128], "Wkv")
        load_w_bf16(Wkv[:, :, 832:1344], w3[:, :, C_DAV:C_DAV + 512], "Wkv")
        S.dma("sp", lambda e: e.dma_start(out=gmix[:], in_=g_mix_d.partition_broadcast(P)), "setup", writes=["gains"])
        load_w_bf16(Wq[:, :, 0:512], w3[:, :, C_DAQ:C_DAQ + 512], "Wq")
        load_w_bf16(Wq[:, :, 512:1024], w3[:, :, C_DSQ:C_DSQ + 512], "Wq")
        load_w_bf16(Wq[:, :, 1024:1536], w3[:, :, C_IXQ:C_IXQ + 512], "Wq")
        load_w_bf16(Wq[:, :, 1536:1544], w3[:, :, C_IXW:C_IXW + 8], "Wq")
        for (src_, a_) in ((pu_d, 0), (pv_d, 1)):
            for r0 in range(0, 16384, 4096):
                S.dma("pool", lambda e: e.dma_start(out=puv_d[r0:r0 + 4096, a_, :], in_=src_[r0:r0 + 4096, :]), "tabcast")


        def a_part1(t):
            b = t % 2
            S.dma("sp", lambda e: e.dma_start(out=xt[b][:], in_=xT[t]), "xld", writes=["xt%d" % b])
            rmsnorm_tile(xt[b][:], "xt%d" % b, gmix, hn[b][:], "hn%d" % b, "A%d" % b, junk, ssq[b], rstd[b])
            transpose_to(hn[b], "hn%d" % b, KC, 0, hnT[b][:].rearrange("p c t -> p (c t)"), "hnT%d" % b)
            S.dma("sp", lambda e: e.dma_start(out=hnT_d[t], in_=hnT[b][:].rearrange("p c t -> p (c t)")), "hnTst",
                  reads=["hnT%d" % b], writes=["hnT_d%d" % t])

        def a_part2(t):
            b = t % 2
            B1 = 1 + 4 * b
            proj(hnT[b], "hnT%d" % b, KC, Wkv, "Wkv", 0, 512, B1)
            proj(hnT[b], "hnT%d" % b, KC, Wkv, "Wkv", 512, 320, B1 + 1)
            proj(hnT[b], "hnT%d" % b, KC, Wkv, "Wkv", 832, 512, B1 + 2)
            flat12 = PS[:, B1:B1 + 2, :].rearrange("p a b -> p (a b)")
            S.op("act", lambda e: e.copy(out=krAll[:], in_=flat12[:, 0:704]), reads=pk(B1, B1 + 1), writes=["krAll"])
            rope4(flat12[:, 0:704].rearrange("p (h d) -> p h d", d=64), krAll[:].rearrange("p (h d) -> p h d", d=64),
                  11, t, ropeA, ropeB, pk(B1, B1 + 1) + ["CC"], "krAll")
            S.op("act", lambda e: e.copy(out=V_ds[:, t, :, 0:64], in_=PS[:, B1 + 1, 192:320].rearrange("p (g d) -> p g d", d=64)),
                 reads=pk(B1 + 1), writes=["Vds"])
            S.op("act", lambda e: e.copy(out=V_da[:, t, :, 0:128], in_=PS[:, B1 + 2, :].rearrange("p (h d) -> p h d", d=128)),
                 reads=pk(B1 + 2), writes=["Vda"])
            for j in range(5):
                S.op("pe", lambda e: e.transpose(out=psbf(4)[:, j * 128:(j + 1) * 128], in_=krAll[:, j * 128:(j + 1) * 128],
                                                 identity=ident[:]), reads=["krAll", "ident"], writes=pk(4))
            S.op("pe", lambda e: e.transpose(out=psbf(4)[0:64, 640:768], in_=krAll[:, 640:704], identity=ident[:]),
                 reads=["krAll", "ident"], writes=pk(4))
            tok = slice(t * 128, (t + 1) * 128)
            S.op("dve", lambda e: e.tensor_copy(out=KT_da[:, :, tok], in_=psbf(4)[:, 0:512].rearrange("p (h t) -> p h t", t=128)),
                 reads=pk(4), writes=["KTda"])
            S.op("dve", lambda e: e.tensor_copy(out=KT_ds[:, tok], in_=psbf(4)[:, 512:640]), reads=pk(4), writes=["KTds"])
            S.op("dve", lambda e: e.tensor_copy(out=KT_ix[0:64, tok], in_=psbf(4)[0:64, 640:768]), reads=pk(4), writes=["KTix"])

        a_part1(0)
        for t in range(NT):
            if t + 1 < NT:
                a_part1(t + 1)
            a_part2(t)
        S.barrier()

    if debug:
        for hh in range(4):
            S.dma("sp", lambda e: e.dma_start(out=dbg["kt"][:, hh * SL:(hh + 1) * SL], in_=KT_da[:, hh, :]), "dbg", reads=["KTda"])

    if stop_after != "A":
      with ExitStack() as es:
        def TT(name, shape, dt):
            return es.enter_context(nc.sbuf_tensor(name, shape, dt))
        hnT = [TT("hnTB%d" % i, [P, KC, 128], BF16) for i in range(1)] * 2
        qall = TT("qall", [P, 1536], BF16)
        QP_ds = TT("QP_ds", [P, 8, 128], BF16)
        QT_da2 = [TT("QT_da%d" % i, [P, 4, 512], BF16) for i in range(2)]
        QT_ds2 = [TT("QT_ds%d" % i, [P, 8, 128], BF16) for i in range(2)]
        QIT = TT("QIT", [P, 8, 128], BF16)
        ixw = TT("ixw", [P, 8], F32)
        absw = TT("absw", [P, 8], F32)
        sgn = TT("sgn", [P, 8], F32)
        rtmp = [TT("rtmp%d" % i, [P, 512], F32) for i in range(2)]
        idx = TT("idx", [P, SL], F32)
        mneg = TT("mneg", [P, SL], BF16)
        maskT2 = [TT("maskT%d" % i, [P, NT, 128], BF16) for i in range(2)]
        PTd = [TT("PTd%d" % i, [P, 8, 128], BF16) for i in range(2)]
        PTa = [TT("PTa%d" % i, [P, 2, 512], BF16) for i in range(2)]
        bl = TT("bl", [P, 8], F32)
        wk = TT("wk", [P, NBIS], F32)
        fvec = TT("fvec", [P, NBIS], F32)
        rcb = TT("rcb", [P, 8], F32)
        o_b = TT("o_b", [P, 512], BF16)
        obT = TT("obT", [P, 4, 128], BF16)
        o_a = TT("o_a", [P, 4, 512], BF16)
        oaT = [TT("oaT%d" % i, [P, 4, 128], BF16) for i in range(2)]
        od = TT("od", [P, 4, 128], F32)
        rca = TT("rca", [P, 8], F32)
        rcl = TT("rcl", [P, 4], F32)
        ssa = TT("ssa", [P, 4], F32)
        junkb = TT("junkb", [P, 128], BF16)

        w3 = w_in_d.rearrange("(c p) n -> p c n", p=P)
        for it_ in range(NBIS):
            S.op("pool", lambda e: e.memset(fvec[:, it_:it_ + 1], 0.5 ** (it_ + 1)), writes=["fvec"])
        S.op("pool", lambda e: e.memset(QP_ds[:], 0.0), writes=["QP_ds"])
        S.op("pool", lambda e: e.memset(QIT[:], 0.0), writes=["QIT"])

        LO, HI, W0, MID, CNT, SELW, THR = range(7)

        def col(i):
            return bl[:, i:i + 1]

        def dsa_tile(qt, hook=None):
            maskT = maskT2[qt % 2]
            mkey = "maskT%d" % (qt % 2)
            kend = (qt + 1) * 128
            nkb = qt + 1
            nch = (kend + 511) // 512
            for c in range(nch):
                n = min(512, kend - c * 512)
                accb = 2 + (c % 2)
                for h in range(8):
                    pb = h % 2
                    S.op("pe", lambda e: e.matmul(PS[:, pb, 0:n], lhsT=QIT[0:64, h, :], rhs=KT_ix[0:64, c * 512:c * 512 + n],
                                                  start=True, stop=True),
                         reads=["QIT", "KTix"], writes=pk(pb))
                    S.op("act", lambda e: e.activation(out=rtmp[pb][:, 0:n], in_=PS[:, pb, 0:n], func=AF.Relu,
                                                       scale=absw[:, h:h + 1]),
                         reads=pk(pb) + ["absw"], writes=["rtmp%d" % pb])
                    if h == 0:
                        S.op("dve", lambda e: e.tensor_scalar(out=PS[:, accb, 0:n], in0=rtmp[pb][:, 0:n], scalar1=sgn[:, 0:1],
                                                              scalar2=None, op0=ALU.mult),
                             reads=["rtmp%d" % pb, "sgn"], writes=pk(accb))
                    else:
                        S.op("dve", lambda e: e.scalar_tensor_tensor(out=PS[:, accb, 0:n], in0=rtmp[pb][:, 0:n],
                                                                     scalar=sgn[:, h:h + 1], in1=PS[:, accb, 0:n],
                                                                     op0=ALU.mult, op1=ALU.add),
                             reads=["rtmp%d" % pb, "sgn"] + pk(accb), writes=pk(accb))
                last = (c == nch - 1)
                n0 = n - 128 if last else n
                if n0 > 0:
                    S.op("act", lambda e: e.copy(out=idx[:, c * 512:c * 512 + n0], in_=PS[:, accb, 0:n0]),
                         reads=pk(accb), writes=["idx"])
                if last:
                    S.op("dve", lambda e: e.tensor_tensor(out=idx[:, kend - 128:kend], in0=PS[:, accb, n0:n], in1=triNEG[:],
                                                          op=ALU.add),
                         reads=pk(accb) + ["triNEG"], writes=["idx"])
            if qt >= 2:
                S.op("dve", lambda e: e.tensor_reduce(out=col(HI), in_=idx[:, 0:kend], axis=AX.X, op=ALU.max),
                     reads=["idx"], writes=["bl"])
                S.op("dve", lambda e: e.tensor_reduce(out=col(LO), in_=idx[:, 0:kend - 128], axis=AX.X, op=ALU.min),
                     reads=["idx", "bl"], writes=["bl"])
                S.op("dve", lambda e: e.tensor_tensor(out=col(W0), in0=col(HI), in1=col(LO), op=ALU.subtract),
                     reads=["bl"], writes=["bl"])
                S.op("dve", lambda e: e.tensor_scalar(out=wk[:], in0=fvec[:], scalar1=col(W0), scalar2=None, op0=ALU.mult),
                     reads=["bl", "fvec"], writes=["wk"])
                S.op("dve", lambda e: e.tensor_tensor(out=col(MID), in0=col(LO), in1=wk[:, 0:1], op=ALU.add),
                     reads=["bl", "wk"], writes=["bl"])
                for it in range(NBIS):
                    S.op("dve", lambda e: e.tensor_scalar(out=mneg[:, 0:kend], in0=idx[:, 0:kend], scalar1=col(MID),
                                                          scalar2=None, op0=ALU.is_ge, op1=ALU.add, accum_out=col(CNT)),
                         reads=["idx", "bl"], writes=["mneg", "bl"])
                    S.op("dve", lambda e: e.tensor_scalar(out=col(SELW), in0=col(CNT), scalar1=float(NSEL) - 0.5, scalar2=0.5,
                                                          op0=ALU.is_ge, op1=ALU.subtract),
                         reads=["bl"], writes=["bl"])
                    if it < NBIS - 1:
                        S.op("dve", lambda e: e.scalar_tensor_tensor(out=col(MID), in0=col(SELW), scalar=wk[:, it:it + 1], in1=col(MID),
                                                                     op0=ALU.mult, op1=ALU.add),
                             reads=["bl", "wk"], writes=["bl"])
                S.op("dve", lambda e: e.tensor_scalar(out=col(SELW), in0=col(SELW), scalar1=-0.5, scalar2=None, op0=ALU.add),
                     reads=["bl"], writes=["bl"])
                S.op("dve", lambda e: e.scalar_tensor_tensor(out=col(LO), in0=col(SELW), scalar=wk[:, NBIS - 1:NBIS], in1=col(MID),
                                                             op0=ALU.mult, op1=ALU.add),
                     reads=["bl", "wk"], writes=["bl"])
                thr = col(LO)
            else:
                S.op("dve", lambda e: e.memset(col(THR), -1.0e29), reads=["bl"], writes=["bl"])
                thr = col(THR)
            S.op("dve", lambda e: e.tensor_scalar(out=mneg[:, 0:kend], in0=idx[:, 0:kend], scalar1=thr, scalar2=NEG,
                                                  op0=ALU.is_lt, op1=ALU.mult),
                 reads=["idx", "bl"], writes=["mneg"])
            if hook is not None:
                hook()
            for k0 in range(0, nkb, 8):
                k1 = min(nkb, k0 + 8)
                tb = 3 - ((k0 // 8) % 2)
                for kb in range(k0, k1):
                    S.op("pe", lambda e: e.transpose(out=psbf(tb)[:, (kb - k0) * 128:(kb - k0 + 1) * 128],
                                                     in_=mneg[:, kb * 128:(kb + 1) * 128], identity=ident[:]),
                         reads=["mneg", "ident"], writes=pk(tb))
                S.op("act", lambda e: e.copy(out=maskT[:, k0:k1, :].rearrange("p k q -> p (k q)"),
                                             in_=psbf(tb)[:, 0:(k1 - k0) * 128]),
                     reads=pk(tb), writes=[mkey])

        def dsa_part2(qt):
            kend = (qt + 1) * 128
            nkb = qt + 1
            maskT = maskT2[qt % 2]
            mkey = "maskT%d" % (qt % 2)
            QT_ds = QT_ds2[qt % 2]
            qdkey = "QT_ds%d" % (qt % 2)
            def dsa_scores(kb):
                sbk = 4 if kb % 2 == 0 else 2
                for g in range(2):
                    S.op("pe", lambda e: e.matmul(PS[:, sbk + g, :], lhsT=KT_ds[:, kb * 128:(kb + 1) * 128],
                                                  rhs=QT_ds[:, 4 * g:4 * g + 4, :].rearrange("p h q -> p (h q)"),
                                                  start=True, stop=False),
                         reads=["KTds", qdkey], writes=pk(sbk + g))
                    S.op("pe", lambda e: e.matmul(PS[:, sbk + g, :].rearrange("p (h q) -> p h q", h=4), lhsT=ident[:],
                                                  rhs=maskT[:, kb, :].unsqueeze(1).to_broadcast([P, 4, 128]), start=False, stop=True),
                         reads=["ident", mkey], writes=pk(sbk + g))
            dsa_scores(0)
            for kb in range(nkb):
                pb2 = kb % 2
                sbk = 4 if kb % 2 == 0 else 2
                if kb + 1 < nkb:
                    dsa_scores(kb + 1)
                S.op("act", lambda e: e.activation(out=PTd[pb2][:].rearrange("p h q -> p (h q)"),
                                                   in_=PS[:, sbk:sbk + 2, :].rearrange("p a b -> p (a b)"), func=AF.Exp, scale=0.125),
                     reads=pk(sbk, sbk + 1), writes=["PTd%d" % pb2])
                for h in range(8):
                    S.op("pe", lambda e: e.matmul(PS[:, 6 + h // 4, (h % 4) * 66:(h % 4) * 66 + 66], lhsT=PTd[pb2][:, h, :],
                                                  rhs=V_ds[:, kb, h // 4, :], start=(kb == 0 and h % 4 == 0),
                                                  stop=(kb == nkb - 1), skip_group_check=True),
                         reads=["PTd%d" % pb2, "Vds"], writes=pk(6 + h // 4))
            pov = PS[:, 6:8, 0:264].rearrange("p b (h e) -> p b h e", e=66)
            S.op("dve", lambda e: e.reciprocal(out=rcb[:].rearrange("p (b h) -> p b h", b=2), in_=pov[:, :, :, 64]),
                 reads=pk(6, 7), writes=["rcb"])
            S.op("dve", lambda e: e.tensor_tensor(out=o_b[:].rearrange("p (b h d) -> p b h d", b=2, h=4),
                                                  in0=pov[:, :, :, 0:64],
                                                  in1=rcb[:].rearrange("p (b h) -> p b h", b=2).unsqueeze(3).to_broadcast([P, 2, 4, 64]),
                                                  op=ALU.mult),
                 reads=pk(6, 7) + ["rcb"], writes=["o_b"])
            if debug:
                S.dma("sp", lambda e: e.dma_start(out=dbg["ob"][qt], in_=o_b[:]), "dbg", reads=["o_b"])
            transpose_to(o_b, "o_b", 4, 3, obT[:].rearrange("p c t -> p (c t)"), "obT")
            S.dma("sp", lambda e: e.dma_start(out=obT_d[qt], in_=obT[:].rearrange("p c t -> p (c t)")), "obTst",
                  reads=["obT"], writes=["obT_d%d" % qt])

        def diff_head(qs, h):
            nkb = 4 * qs + 4
            QT_da = QT_da2[qs % 2]
            qkey = "QT_da%d" % (qs % 2)
            if True:
                def da_scores(kb):
                    j = kb - 4 * qs
                    q0 = max(0, j) * 128
                    sb = kb % 2
                    for m in range(2):
                        S.op("pe", lambda e: e.matmul(PS[:, 2 * sb + m, q0:512], lhsT=KT_da[64 * m:64 * m + 64, h, kb * 128:(kb + 1) * 128],
                                                      rhs=QT_da[64 * m:64 * m + 64, h, q0:512], start=True, stop=True),
                             reads=["KTda", qkey], writes=pk(2 * sb + m))
                da_scores(0)
                for kb in range(nkb):
                    j = kb - 4 * qs
                    jj = max(0, j)
                    q0 = jj * 128
                    sb = kb % 2
                    if kb + 1 < nkb:
                        da_scores(kb + 1)
                    S.op("act", lambda e: e.activation(out=PTa[sb][:, :, q0:512], in_=PS[:, 2 * sb:2 * sb + 2, q0:512],
                                                       func=AF.Exp, scale=0.125),
                         reads=pk(2 * sb, 2 * sb + 1), writes=["PTa%d" % sb])
                    if j >= 0:
                        S.op("pool", lambda e: e.tensor_tensor(out=PTa[sb][:, :, q0:q0 + 128], in0=PTa[sb][:, :, q0:q0 + 128],
                                                               in1=tri01[:].unsqueeze(1).to_broadcast([P, 2, 128]), op=ALU.mult),
                             reads=["PTa%d" % sb, "tri01"], writes=["PTa%d" % sb])
                    for m in range(2):
                        for qi in range(jj, 4):
                            r = m * 4 + qi
                            S.op("pe", lambda e: e.matmul(PS[:, 4 + r // 2, (r % 2) * 130:(r % 2) * 130 + 130],
                                                          lhsT=PTa[sb][:, m, qi * 128:(qi + 1) * 128], rhs=V_da[:, kb, h, :],
                                                          start=(kb == 0 and r % 2 == 0), stop=(kb == 4 * qs + qi),
                                                          skip_group_check=True),
                                 reads=["PTa%d" % sb, "Vda"], writes=pk(4 + r // 2))
                pov = PS[:, 4:8, 0:260].rearrange("p b (s e) -> p b s e", e=130)
                S.op("dve", lambda e: e.reciprocal(out=rca[:].rearrange("p (b s) -> p b s", s=2), in_=pov[:, :, :, 128]),
                     reads=pk(4, 5, 6, 7), writes=["rca"])
                S.op("dve", lambda e: e.tensor_scalar(out=rcl[:], in0=rca[:, 4:8], scalar1=neglam[:, 0:1], scalar2=None,
                                                      op0=ALU.mult), reads=["rca", "neglam"], writes=["rcl"])
                for qi in range(4):
                    r0, r1 = qi, 4 + qi
                    S.op("dve", lambda e: e.tensor_scalar(out=od[:, qi, :], in0=PS[:, 4 + r0 // 2, (r0 % 2) * 130:(r0 % 2) * 130 + 128],
                                                          scalar1=rca[:, r0:r0 + 1], scalar2=None, op0=ALU.mult),
                         reads=pk(4 + r0 // 2) + ["rca"], writes=["od%d" % qi])
                    S.op("dve", lambda e: e.scalar_tensor_tensor(out=od[:, qi, :],
                                                                 in0=PS[:, 4 + r1 // 2, (r1 % 2) * 130:(r1 % 2) * 130 + 128],
                                                                 scalar=rcl[:, qi:qi + 1], in1=od[:, qi, :], op0=ALU.mult, op1=ALU.add),
                         reads=pk(4 + r1 // 2) + ["rcl", "od%d" % qi], writes=["od%d" % qi])
                    S.op("act", lambda e: e.activation(out=junkb[:], in_=od[:, qi, :], func=AF.Square, accum_out=ssa[:, qi:qi + 1]),
                         reads=["od%d" % qi], writes=["junkb", "ssa%d" % qi])
                S.op("act", lambda e: e.activation(out=ssa[:], in_=ssa[:], func=AF.Sqrt, scale=1.0 / 128, bias=EPS),
                     reads=["ssa%d" % i for i in range(4)], writes=["ssa"])
                S.op("dve", lambda e: e.reciprocal(out=ssa[:], in_=ssa[:]), reads=["ssa"], writes=["ssa"] + ["ssa%d" % i for i in range(4)])
                for qi in range(4):
                    S.op("dve", lambda e: e.scalar_tensor_tensor(out=o_a[:, qi, h * 128:(h + 1) * 128], in0=od[:, qi, :],
                                                                 scalar=ssa[:, qi:qi + 1], in1=subg08[:], op0=ALU.mult, op1=ALU.mult),
                         reads=["od%d" % qi, "ssa", "subg08"], writes=["o_a"])

        def diff_finalize(qs):
            for qi in range(4):
                qt = qs * 4 + qi
                ob_ = oaT[qi % 2]
                if debug:
                    S.dma("sp", lambda e: e.dma_start(out=dbg["oa"][qt], in_=o_a[:, qi, :]), "dbg", reads=["o_a"])
                for hh in range(4):
                    S.op("pe", lambda e: e.transpose(out=psbf(qi % 2)[:, hh * 128:(hh + 1) * 128],
                                                     in_=o_a[:, qi, hh * 128:(hh + 1) * 128], identity=ident[:]),
                         reads=["o_a", "ident"], writes=pk(qi % 2))
                S.op("act", lambda e: e.copy(out=ob_[:].rearrange("p c t -> p (c t)"), in_=psbf(qi % 2)[:, 0:512]),
                     reads=pk(qi % 2), writes=["oaT%d" % (qi % 2)])
                S.dma("sp", lambda e: e.dma_start(out=oaT_d[qt], in_=ob_[:].rearrange("p c t -> p (c t)")), "oaTst",
                      reads=["oaT%d" % (qi % 2)], writes=["oaT_d%d" % qt])

        nqs = 8

        def qproj(qt):
            qs, qi = qt // 4, qt % 4
            b = qt % 2
            S.dma("sp", lambda e: e.dma_start(out=hnT[b][:].rearrange("p c t -> p (c t)"), in_=hnT_d[qt]), "hnTld",
                  reads=["hnT_d%d" % qt], writes=["hnTB"])
            proj(hnT[b], "hnTB", KC, Wq, "Wq", 0, 512, 0)
            proj(hnT[b], "hnTB", KC, Wq, "Wq", 512, 512, 1)
            proj(hnT[b], "hnTB", KC, Wq, "Wq", 1024, 512, 2)
            proj(hnT[b], "hnTB", KC, Wq, "Wq", 1536, 8, 3)
            flat = PS[:, 0:3, :].rearrange("p a b -> p (a b)")
            S.op("act", lambda e: e.copy(out=qall[:], in_=flat), reads=pk(0, 1, 2), writes=["qall"])
            rope4(flat.rearrange("p (h d) -> p h d", d=64), qall[:].rearrange("p (h d) -> p h d", d=64), 24, qt,
                  ropeA, ropeB, pk(0, 1, 2) + ["CC"], "qall")
            S.op("dve", lambda e: e.tensor_copy(out=ixw[:], in_=PS[:, 3, 0:8]), reads=pk(3), writes=["ixw"])
            S.op("dve", lambda e: e.scalar_tensor_tensor(out=absw[:], in0=ixw[:], scalar=-1.0, in1=ixw[:], op0=ALU.mult, op1=ALU.max),
                 reads=["ixw"], writes=["absw"])
            S.op("dve", lambda e: e.tensor_scalar(out=sgn[:], in0=ixw[:], scalar1=0.0, scalar2=2.0, op0=ALU.is_ge, op1=ALU.mult),
                 reads=["ixw"], writes=["sgn"])
            S.op("dve", lambda e: e.tensor_scalar(out=sgn[:], in0=sgn[:], scalar1=-1.0, scalar2=None, op0=ALU.add),
                 reads=["sgn"], writes=["sgn"])
            S.op("pool", lambda e: e.tensor_copy(out=QP_ds[:, 0:4, 0:64], in_=qall[:, 512:768].rearrange("p (h d) -> p h d", d=64)),
                 reads=["qall"], writes=["QP_ds"])
            S.op("pool", lambda e: e.tensor_copy(out=QP_ds[:, 4:8, 64:128], in_=qall[:, 768:1024].rearrange("p (h d) -> p h d", d=64)),
                 reads=["qall", "QP_ds"], writes=["QP_ds"])
            for j in range(4):
                S.op("pe", lambda e: e.transpose(out=psbf(4)[:, j * 128:(j + 1) * 128], in_=qall[:, j * 128:(j + 1) * 128],
                                                 identity=ident[:]), reads=["qall", "ident"], writes=pk(4))
            for h in range(8):
                S.op("pe", lambda e: e.transpose(out=psbf(5)[:, h * 128:(h + 1) * 128], in_=QP_ds[:, h, :], identity=ident[:]),
                     reads=["QP_ds", "ident"], writes=pk(5))
            for h in range(8):
                S.op("pe", lambda e: e.transpose(out=psbf(6)[0:64, h * 128:(h + 1) * 128],
                                                 in_=qall[:, 1024 + h * 64:1024 + (h + 1) * 64], identity=ident[:]),
                     reads=["qall", "ident"], writes=pk(6))
            S.op("act", lambda e: e.copy(out=QT_da2[qs % 2][:, :, qi * 128:(qi + 1) * 128],
                                         in_=psbf(4)[:, 0:512].rearrange("p (h t) -> p h t", t=128)),
                 reads=pk(4), writes=["QT_da%d" % (qs % 2)])
            S.op("dve", lambda e: e.tensor_copy(out=QT_ds2[qt % 2][:].rearrange("p h t -> p (h t)"), in_=psbf(5)[:, :]),
                 reads=pk(5), writes=["QT_ds%d" % (qt % 2)])
            S.op("act", lambda e: e.copy(out=QIT[0:64, :, :].rearrange("p h t -> p (h t)"), in_=psbf(6)[0:64, :]),
                 reads=pk(6), writes=["QIT"])

        qproj(0)
        for qs in range(nqs):
            for qi in range(4):
                qt = qs * 4 + qi
                def hook(qs=qs, qi=qi, qt=qt):
                    if qt >= 1:
                        dsa_part2(qt - 1)
                    if qs >= 1:
                        diff_head(qs - 1, qi)
                    if qt + 1 < 4 * nqs:
                        qproj(qt + 1)
                dsa_tile(qt, hook=hook)
            if qs >= 1:
                diff_finalize(qs - 1)
        dsa_part2(4 * nqs - 1)
        for h in range(4):
            diff_head(nqs - 1, h)
        diff_finalize(nqs - 1)
        S.barrier()
    kv.close()

    if stop_after not in ("A", "B"):
      with ExitStack() as es:
        def TT(name, shape, dt):
            return es.enter_context(nc.sbuf_tensor(name, shape, dt))
        Wg = TT("Wg", [P, KC, 2048], BF16)
        Wa = TT("Wa", [P, 4, D], BF16)
        Wb = TT("Wb", [P, 4, D], BF16)
        Wo = TT("Wo", [P, KC, D], BF16)
        Wmq = TT("Wmq", [P, KC, 512], BF16)
        Wmo = TT("Wmo", [P, 4, D], BF16)
        gbp = TT("gbp", [P, 2048], BF16)
        onesp = TT("onesp", [P, P], BF16)
        gmem = TT("gmem", [P, D], F32)
        KmT = TT("KmT", [P, 4, 256], BF16)
        Vm = TT("Vm", [P, 2, 4, 130], BF16)

        S.op("pool", lambda e: e.memset(gbp[:], 0.0), writes=["gbp"])
        S.dma("pool", lambda e: e.dma_start(out=gbp[0:1, :], in_=gbias_d.rearrange("(o n) -> o n", o=1)), "wload",
              reads=["gbp"], writes=["gbp"])
        S.op("pool", lambda e: e.memset(onesp[:], 0.0), writes=["onesp"])
        S.op("pool", lambda e: e.memset(onesp[0:1, :], 1.0), reads=["onesp"], writes=["onesp"])
        S.dma("sp", lambda e: e.dma_start(out=gmem[:], in_=g_mem_d.partition_broadcast(P)), "setup", writes=["gains"])
        S.op("pool", lambda e: e.memset(Vm[:, :, :, 128:130], 1.0), writes=["Vm"])

        with ExitStack() as es2:
            def T2(name, shape, dt):
                return es2.enter_context(nc.sbuf_tensor(name, shape, dt))
            Wmkv = T2("Wmkv", [P, KC, D], BF16)
            gkv = T2("gkv", [P, D], F32)
            mt = T2("mt", [P, D], F32)
            junk0 = T2("junk0", [P, D], BF16)
            ssq0 = T2("ssq0", [P, 1], F32)
            rstd0 = T2("rstd0", [P, 1], F32)
            hm0 = T2("hm0", [P, D], BF16)
            hmT0 = T2("hmT0", [P, KC, 128], BF16)
            load_w_bf16(Wmkv[:], wmkv_d.rearrange("(c p) n -> p c n", p=P), "Wmkv")
            load_w_bf16(Wg[:], w_in_d.rearrange("(c p) n -> p c n", p=P)[:, :, C_GATE:C_GATE + 2048], "Wg")
            load_w_bf16(Wa[:], wa_d.rearrange("(c p) n -> p c n", p=P), "Wa")
            load_w_bf16(Wb[:], wb_d.rearrange("(c p) n -> p c n", p=P), "Wb")
            load_w_bf16(Wo[:], wo_d.rearrange("(c p) n -> p c n", p=P), "Wo")
            load_w_bf16(Wmq[:], wmq_d.rearrange("(c p) n -> p c n", p=P), "Wmq")
            load_w_bf16(Wmo[:], wmo_d.rearrange("(c p) n -> p c n", p=P), "Wmo")
            S.dma("sp", lambda e: e.dma_start(out=gkv[:], in_=g_kv_d.partition_broadcast(P)), "setup", writes=["gainkv"])
            memT = mem_d.rearrange("(t p) d -> t p d", p=P)
            for mb in range(2):
                S.dma("sp", lambda e: e.dma_start(out=mt[:], in_=memT[mb]), "xld", writes=["mt"])
                rmsnorm_tile(mt[:], "mt", gkv, hm0[:], "hm0", "0", junk0, ssq0, rstd0, gkey="gainkv")
                transpose_to(hm0, "hm0", KC, 0, hmT0[:].rearrange("p c t -> p (c t)"), "hmT0")
                for hh in range(4):
                    for c in range(KC):
                        S.op("pe", lambda e: e.matmul(PS[:, 1, hh * 128:(hh + 1) * 128], lhsT=Wmkv[:, c, hh * 128:(hh + 1) * 128],
                                                      rhs=hmT0[:, c, :], start=(c == 0 and hh == 0), stop=(c == KC - 1),
                                                      skip_group_check=True),
                             reads=["Wmkv", "hmT0"], writes=pk(1))
                S.op("act", lambda e: e.copy(out=KmT[:, :, mb * 128:(mb + 1) * 128], in_=PS[:, 1, :].rearrange("p (h m) -> p h m", m=128)),
                     reads=pk(1), writes=["KmT"])
                proj(hmT0, "hmT0", KC, Wmkv, "Wmkv", 512, 512, 2)
                S.op("act", lambda e: e.copy(out=Vm[:, mb, :, 0:128], in_=PS[:, 2, :].rearrange("p (h d) -> p h d", d=128)),
                     reads=pk(2), writes=["Vm"])
            S.barrier()

        def T2x(name, shape, dt):
            return [TT("%s_%d" % (name, i), shape, dt) for i in range(2)]
        xt = T2x("xtC", [P, D], F32)
        hnT = T2x("hnTC", [P, KC, 128], BF16)
        oaT = T2x("oaTC", [P, 4, 128], BF16)
        obT = T2x("obTC", [P, 4, 128], BF16)
        gsig = T2x("gsig", [P, 2048], F32)
        t1 = T2x("t1", [P, D], F32)
        t2 = T2x("t2", [P, D], F32)
        mix = T2x("mix", [P, D], BF16)
        mixT = T2x("mixT", [P, KC, 128], BF16)
        x1 = T2x("x1", [P, D], F32)
        junk = T2x("junkC", [P, D], BF16)
        ssq = T2x("ssqC", [P, 1], F32)
        rstd = T2x("rstdC", [P, 1], F32)
        hm = T2x("hm", [P, D], BF16)
        hmT = T2x("hmT", [P, KC, 128], BF16)
        qmT = T2x("qmT", [P, 4, 128], BF16)
        PTm = T2x("PTm", [P, 8, 128], BF16)
        rcm = T2x("rcm", [P, 4], F32)
        om = T2x("om", [P, 512], BF16)
        omT = T2x("omT", [P, 4, 128], BF16)
        x2 = T2x("x2C", [P, D], F32)

        def c1_steps(t):
            p = t % 2
            B0, B1, B2, B3 = 4 * p, 4 * p + 1, 4 * p + 2, 4 * p + 3
            sx = "_%d" % p
            st = []
            A = st.append

            def loads():
                S.dma("sp", lambda e: e.dma_start(out=xt[p][:], in_=xT[t]), "xld", writes=["xt" + sx])
                S.dma("sp", lambda e: e.dma_start(out=hnT[p][:].rearrange("p c t -> p (c t)"), in_=hnT_d[t]), "hnTld", writes=["hnT" + sx])
                S.dma("sp", lambda e: e.dma_start(out=oaT[p][:].rearrange("p c t -> p (c t)"), in_=oaT_d[t]), "oaTld", writes=["oaT" + sx])
                S.dma("sp", lambda e: e.dma_start(out=obT[p][:].rearrange("p c t -> p (c t)"), in_=obT_d[t]), "obTld", writes=["obT" + sx])
            A(loads)

            def gates(r):
                for gq in (2 * r, 2 * r + 1):
                    bank = (B0, B1, B2, B3)[gq]
                    for c in range(KC):
                        S.op("pe", lambda e: e.matmul(PS[:, bank, :], lhsT=hnT[p][:, c, :], rhs=Wg[:, c, gq * 512:(gq + 1) * 512],
                                                      start=(c == 0), stop=False), reads=["hnT" + sx, "Wg"], writes=pk(bank))
                    S.op("pe", lambda e: e.matmul(PS[:, bank, :], lhsT=onesp[:], rhs=gbp[:, gq * 512:(gq + 1) * 512], start=False, stop=True),
                         reads=["onesp", "gbp"], writes=pk(bank))
                bk = B0 if r == 0 else B2
                S.op("act", lambda e: e.activation(out=gsig[p][:, r * D:(r + 1) * D], in_=PS[:, bk:bk + 2, :].rearrange("p a b -> p (a b)"),
                                                   func=AF.Sigmoid),
                     reads=pk(bk, bk + 1), writes=["gsig%d" % r + sx])
            A(lambda: gates(0))
            A(lambda: gates(1))

            def branch(r):
                W_, src, skey = (Wa, oaT[p], "oaT" + sx) if r == 0 else (Wb, obT[p], "obT" + sx)
                bk = B0 if r == 0 else B2
                for hf in range(2):
                    for c in range(4):
                        S.op("pe", lambda e: e.matmul(PS[:, bk + hf, :], lhsT=src[:, c, :], rhs=W_[:, c, hf * 512:(hf + 1) * 512],
                                                      start=(c == 0), stop=(c == 3)), reads=[skey, "Wa" if r == 0 else "Wb"], writes=pk(bk + hf))
                dst = t1[p] if r == 0 else t2[p]
                S.op("dve", lambda e: e.tensor_tensor(out=dst[:], in0=PS[:, bk:bk + 2, :].rearrange("p a b -> p (a b)"),
                                                      in1=gsig[p][:, r * D:(r + 1) * D], op=ALU.mult),
                     reads=pk(bk, bk + 1) + ["gsig%d" % r + sx], writes=["t%d" % r + sx])
            A(lambda: branch(0))
            A(lambda: branch(1))
            A(lambda: S.op("pool", lambda e: e.tensor_tensor(out=mix[p][:], in0=t1[p][:], in1=t2[p][:], op=ALU.add),
                           reads=["t0" + sx, "t1" + sx], writes=["mix" + sx]))
            A(lambda: transpose_to(mix[p], "mix" + sx, KC, B0, mixT[p][:].rearrange("p c t -> p (c t)"), "mixT" + sx))

            def outproj():
                proj(mixT[p], "mixT" + sx, KC, Wo, "Wo", 0, 512, B1)
                proj(mixT[p], "mixT" + sx, KC, Wo, "Wo", 512, 512, B2)
                S.op("dve", lambda e: e.tensor_tensor(out=x1[p][:], in0=PS[:, B1:B1 + 2, :].rearrange("p a b -> p (a b)"), in1=xt[p][:], op=ALU.add),
                     reads=pk(B1, B2) + ["xt" + sx], writes=["x1" + sx])
                if debug:
                    S.dma("sp", lambda e: e.dma_start(out=dbg["x1"][t], in_=x1[p][:]), "dbg", reads=["x1" + sx])
            A(outproj)
            A(lambda: rmsnorm_tile(x1[p][:], "x1" + sx, gmem, hm[p][:], "hm" + sx, "C" + sx, junk[p], ssq[p], rstd[p]))
            A(lambda: transpose_to(hm[p], "hm" + sx, KC, B3, hmT[p][:].rearrange("p c t -> p (c t)"), "hmT" + sx))

            def qproj():
                for hh in range(4):
                    for c in range(KC):
                        S.op("pe", lambda e: e.matmul(PS[:, B0, hh * 128:(hh + 1) * 128], lhsT=Wmq[:, c, hh * 128:(hh + 1) * 128],
                                                      rhs=hmT[p][:, c, :], start=(c == 0 and hh == 0), stop=(c == KC - 1),
                                                      skip_group_check=True),
                             reads=["Wmq", "hmT" + sx], writes=pk(B0))
                S.op("act", lambda e: e.copy(out=qmT[p][:].rearrange("p h t -> p (h t)"), in_=PS[:, B0, :]), reads=pk(B0), writes=["qmT" + sx])
            A(qproj)

            def scores():
                for hh in range(4):
                    for mb in range(2):
                        r = hh * 2 + mb
                        bank = B1 + r // 4
                        S.op("pe", lambda e: e.matmul(PS[:, bank, (r % 4) * 128:(r % 4 + 1) * 128],
                                                      lhsT=KmT[:, hh, mb * 128:(mb + 1) * 128], rhs=qmT[p][:, hh, :],
                                                      start=(r % 4 == 0), stop=True, skip_group_check=True),
                             reads=["KmT", "qmT" + sx], writes=pk(bank))
                S.op("act", lambda e: e.activation(out=PTm[p][:].rearrange("p r t -> p (r t)"), in_=PS[:, B1:B1 + 2, :].rearrange("p a b -> p (a b)"),
                                                   func=AF.Exp, scale=float(128 ** -0.5)),
                     reads=pk(B1, B2), writes=["PTm" + sx])
            A(scores)

            def pv():
                for hh in range(4):
                    bank = B3 if hh < 2 else B0
                    for mb in range(2):
                        S.op("pe", lambda e: e.matmul(PS[:, bank, (hh % 2) * 130:(hh % 2) * 130 + 130],
                                                      lhsT=PTm[p][:, hh * 2 + mb, :], rhs=Vm[:, mb, hh, :],
                                                      start=(mb == 0 and hh % 2 == 0), stop=(mb == 1), skip_group_check=True),
                             reads=["PTm" + sx, "Vm"], writes=pk(bank))
                for half, bank in ((0, B3), (1, B0)):
                    pv_ = PS[:, bank, 0:260].rearrange("p (s e) -> p s e", e=130)
                    S.op("dve", lambda e: e.reciprocal(out=rcm[p][:, 2 * half:2 * half + 2], in_=pv_[:, :, 128]),
                         reads=pk(bank), writes=["rcm%d" % half + sx])
                    S.op("dve", lambda e: e.tensor_tensor(out=om[p][:, half * 256:(half + 1) * 256].rearrange("p (s d) -> p s d", s=2),
                                                          in0=pv_[:, :, 0:128],
                                                          in1=rcm[p][:, 2 * half:2 * half + 2].unsqueeze(2).to_broadcast([P, 2, 128]),
                                                          op=ALU.mult),
                         reads=pk(bank) + ["rcm%d" % half + sx], writes=["om" + sx])
            A(pv)
            A(lambda: transpose_to(om[p], "om" + sx, 4, B1, omT[p][:].rearrange("p c t -> p (c t)"), "omT" + sx))

            def oproj():
                for hf in range(2):
                    for c in range(4):
                        S.op("pe", lambda e: e.matmul(PS[:, B2 + hf, :], lhsT=omT[p][:, c, :], rhs=Wmo[:, c, hf * 512:(hf + 1) * 512],
                                                      start=(c == 0), stop=(c == 3)), reads=["omT" + sx, "Wmo"], writes=pk(B2 + hf))
                S.op("dve", lambda e: e.tensor_tensor(out=x2[p][:], in0=PS[:, B2:B2 + 2, :].rearrange("p a b -> p (a b)"), in1=x1[p][:], op=ALU.add),
                     reads=pk(B2, B3) + ["x1" + sx], writes=["x2" + sx])
                S.dma("sp", lambda e: e.dma_start(out=x2_d[t], in_=x2[p][:]), "x2st", reads=["x2" + sx], writes=["x2_d%d" % t])
                if debug:
                    S.dma("sp", lambda e: e.dma_start(out=dbg["x2"][t], in_=x2[p][:]), "dbg", reads=["x2" + sx])
            A(oproj)
            return st

        Acur = c1_steps(0)
        hsplit = 7
        for f_ in Acur[:hsplit]:
            f_()
        for t in range(NT):
            Bn = c1_steps(t + 1) if t + 1 < NT else []
            i, j = hsplit, 0
            jmax = min(hsplit, len(Bn))
            while i < len(Acur) or j < jmax:
                if i < len(Acur):
                    Acur[i]()
                    i += 1
                if j < jmax:
                    Bn[j]()
                    j += 1
            Acur = Bn
        S.barrier()

    if stop_after is None:
      with ExitStack() as es:
        def TT(name, shape, dt):
            return es.enter_context(nc.sbuf_tensor(name, shape, dt))
        Wpq = TT("Wpq", [P, KC, D], BF16)
        skT = TT("skT", [P, 8, 128], BF16)
        gffn = TT("gffn", [P, D], F32)
        gfin = TT("gfin", [P, D], F32)
        iota16 = TT("iota16", [P, 16], F32)
        x2 = [TT("x2P%d" % i, [P, D], F32) for i in range(2)]
        junk = TT("junkP", [P, D], BF16)
        junk2 = TT("junkP2", [P, D], BF16)
        junk3 = TT("junkP3", [P, D], BF16)
        DVEACC = 1000000
        ssq = TT("ssqP", [P, 1], F32)
        rstd = TT("rstdP", [P, 1], F32)
        ssq2 = TT("ssqP2", [P, 1], F32)
        rstd2 = TT("rstdP2", [P, 1], F32)
        h3 = TT("h3", [P, D], F32)
        h3b = [TT("h3b%d" % i, [P, D], BF16) for i in range(2)]
        h3T = TT("h3T", [P, KC, 128], BF16)
        pqT = TT("pqT", [P, 8, 128], BF16)
        scs = TT("scs", [P, 2, 8, 128], F32)
        scw = TT("scw", [P, 2, 8, 128], F32)
        tv = TT("tv", [P, 2, 8, 16], F32)
        tiu = TT("tiu", [P, 2, 8, 16], U32)
        tif = TT("tif", [P, 2, 8, 16], F32)
        cand = TT("cand", [P, 8, 16, 16], F32)
        candw = TT("candw", [P, 8, 16, 16], F32)
        bs = TT("bs", [P, 8, 16], F32)
        bpu = TT("bpu", [P, 8, 16], U32)
        bpi = TT("bpi", [P, 8, 16], U32)
        bpj = TT("bpj", [P, 8, 16], U32)
        bif = TT("bif", [P, 8, 16], F32)
        bjf = TT("bjf", [P, 8, 16], F32)
        eq = TT("eq", [P, 128, 16], F32)
        k1f = TT("k1f", [P, 128], F32)
        k2f = TT("k2f", [P, 128], F32)
        eidx = [TT("eidx%d" % i, [P, 128], I32) for i in range(2)]
        gate = [TT("gate%d" % i, [P, 8, 16], F32) for i in range(2)]
        gsum = TT("gsum", [P, 8], F32)
        aval = TT("aval", [P, 128], F32)
        gl = TT("gl", [P, 128], F32)
        NG = 16
        uv = [TT("uv%d" % i, [P, 2 * D], BF16) for i in range(NG)]
        NPR = 6
        prod = [TT("prod%d" % i, [P, D], BF16) for i in range(NPR)]
        dg = [TT("dg%d" % i, [P, P], BF16) for i in range(4)]
        x3 = TT("x3", [P, D], F32)
        ot = [TT("ot%d" % i, [P, D], F32) for i in range(2)]
        puv2 = puv_d.rearrange("e a d -> e (a d)")

        load_w_bf16(Wpq[:], wpq_d.rearrange("(c p) n -> p c n", p=P), "Wpq")
        load_w_bf16(skT[:], skt_d, "skT")
        S.dma("sp", lambda e: e.dma_start(out=gffn[:], in_=g_ffn_d.partition_broadcast(P)), "setup", writes=["gains"])
        S.dma("sp", lambda e: e.dma_start(out=gfin[:], in_=g_fin_d.partition_broadcast(P)), "setup", writes=["gainf"])
        S.op("pool", lambda e: e.iota(eidx[0][:, 0:16], pattern=[[1, 16]], base=0, channel_multiplier=0), writes=["eidx0"])
        S.op("dve", lambda e: e.tensor_copy(out=iota16[:], in_=eidx[0][:, 0:16]), reads=["eidx0"], writes=["iota16"])

        def topk_steps(t):
            b = t % 2
            st = []
            A = st.append
            A(lambda: S.dma("sp", lambda e: e.dma_start(out=x2[b][:], in_=x2_d[t]), "x2ld", writes=["x2%d" % b]))
            A(lambda: rmsnorm_tile(x2[b][:], "x2%d" % b, gffn, h3[:], "h3", "P", junk, ssq, rstd))
            A(lambda: S.op("act", lambda e: e.copy(out=h3b[b][:], in_=h3[:]), reads=["h3"], writes=["h3b%d" % b]))
            A(lambda: transpose_to(h3b[b], "h3b%d" % b, KC, 0, h3T[:].rearrange("p c t -> p (c t)"), "h3T"))

            def qT(hh):
                for c in range(KC):
                    S.op("pe", lambda e: e.matmul(PS[:, 1 + hh // 4, (hh % 4) * 128:(hh % 4 + 1) * 128],
                                                  lhsT=Wpq[:, c, hh * 128:(hh + 1) * 128], rhs=h3T[:, c, :],
                                                  start=(c == 0 and hh % 4 == 0), stop=(c == KC - 1), skip_group_check=True),
                         reads=["Wpq", "h3T"], writes=pk(1 + hh // 4))
            for hh in range(8):
                A(lambda hh=hh: qT(hh))
            A(lambda: S.op("act", lambda e: e.copy(out=pqT[:].rearrange("p h t -> p (h t)"), in_=PS[:, 1:3, :].rearrange("p a b -> p (a b)")),
                           reads=pk(1, 2), writes=["pqT"]))

            def sc(c2):
                for hh in range(8):
                    bank = (0 if c2 == 0 else 2) + hh // 4
                    S.op("pe", lambda e: e.matmul(PS[:, bank, (hh % 4) * 128:(hh % 4 + 1) * 128],
                                                  lhsT=pqT[64 * c2:64 * c2 + 64, hh, :], rhs=skT[64 * c2:64 * c2 + 64, hh, :],
                                                  start=(hh % 4 == 0), stop=True, skip_group_check=True),
                         reads=["pqT", "skT"], writes=pk(bank))
                bk = 0 if c2 == 0 else 2
                S.op("act", lambda e: e.copy(out=scs[:, c2, :, :].rearrange("p h k -> p (h k)"),
                                             in_=PS[:, bk:bk + 2, :].rearrange("p a b -> p (a b)")),
                     reads=pk(bk, bk + 1), writes=["scs%d" % c2])
            A(lambda: sc(0))
            A(lambda: sc(1))

            grp = [(c2, hh) for c2 in range(2) for hh in range(8)]

            def t16(stage, gs):
                for (c2, hh) in gs:
                    g = c2 * 8 + hh
                    sk = "scs%d" % c2
                    if stage == 0:
                        S.op("dve", lambda e: e.max(out=tv[:, c2, hh, 0:8], in_=scs[:, c2, hh, :]), reads=[sk], writes=["tva%d" % g])
                    elif stage == 1:
                        S.op("dve", lambda e: e.max_index(out=tiu[:, c2, hh, 0:8], in_max=tv[:, c2, hh, 0:8], in_values=scs[:, c2, hh, :]),
                             reads=[sk, "tva%d" % g], writes=["tiua%d" % g])
                    elif stage == 2:
                        S.op("dve", lambda e: e.match_replace(out=scw[:, c2, hh, :], in_to_replace=tv[:, c2, hh, 0:8],
                                                              in_values=scs[:, c2, hh, :], imm_value=NEG),
                             reads=[sk, "tva%d" % g], writes=["scw%d" % g])
                    elif stage == 3:
                        S.op("dve", lambda e: e.max(out=tv[:, c2, hh, 8:16], in_=scw[:, c2, hh, :]), reads=["scw%d" % g], writes=["tvb%d" % g])
                    else:
                        S.op("dve", lambda e: e.max_index(out=tiu[:, c2, hh, 8:16], in_max=tv[:, c2, hh, 8:16], in_values=scw[:, c2, hh, :]),
                             reads=["scw%d" % g, "tvb%d" % g], writes=["tiub%d" % g])
            for stage in range(5):
                for half in range(2):
                    A(lambda stage=stage, half=half: t16(stage, grp[half * 8:(half + 1) * 8]))
            tvk = ["tva%d" % g for g in range(16)] + ["tvb%d" % g for g in range(16)]
            tik = ["tiua%d" % g for g in range(16)] + ["tiub%d" % g for g in range(16)]
            A(lambda: S.op("dve", lambda e: e.tensor_copy(out=tif[:], in_=tiu[:]), reads=tik, writes=["tif"]))
            A(lambda: S.op("dve", lambda e: e.tensor_tensor(out=cand[:], in0=tv[:, 0, :, :].unsqueeze(3).to_broadcast([P, 8, 16, 16]),
                                                            in1=tv[:, 1, :, :].unsqueeze(2).to_broadcast([P, 8, 16, 16]), op=ALU.add),
                           reads=tvk, writes=["cand"]))

            def b16(stage):
                for hh in range(8):
                    cv = cand[:, hh, :, :].rearrange("p i j -> p (i j)")
                    cw = candw[:, hh, :, :].rearrange("p i j -> p (i j)")
                    if stage == 0:
                        S.op("dve", lambda e: e.max(out=bs[:, hh, 0:8], in_=cv), reads=["cand"], writes=["bsa%d" % hh])
                    elif stage == 1:
                        S.op("dve", lambda e: e.max_index(out=bpu[:, hh, 0:8], in_max=bs[:, hh, 0:8], in_values=cv),
                             reads=["cand", "bsa%d" % hh], writes=["bpua%d" % hh])
                    elif stage == 2:
                        S.op("dve", lambda e: e.match_replace(out=cw, in_to_replace=bs[:, hh, 0:8], in_values=cv, imm_value=NEG),
                             reads=["cand", "bsa%d" % hh], writes=["candw%d" % hh])
                    elif stage == 3:
                        S.op("dve", lambda e: e.max(out=bs[:, hh, 8:16], in_=cw), reads=["candw%d" % hh], writes=["bsb%d" % hh])
                    else:
                        S.op("dve", lambda e: e.max_index(out=bpu[:, hh, 8:16], in_max=bs[:, hh, 8:16], in_values=cw),
                             reads=["candw%d" % hh, "bsb%d" % hh], writes=["bpub%d" % hh])
            for stage in range(5):
                A(lambda stage=stage: b16(stage))
            bsk = ["bsa%d" % h for h in range(8)] + ["bsb%d" % h for h in range(8)]
            bpk = ["bpua%d" % h for h in range(8)] + ["bpub%d" % h for h in range(8)]

            def posij():
                S.op("dve", lambda e: e.tensor_single_scalar(out=bpi[:], in_=bpu[:], scalar=4, op=ALU.logical_shift_right),
                     reads=bpk, writes=["bpi"])
                S.op("dve", lambda e: e.tensor_single_scalar(out=bpj[:], in_=bpu[:], scalar=15, op=ALU.bitwise_and),
                     reads=bpk, writes=["bpj"])
                S.op("dve", lambda e: e.tensor_copy(out=bif[:], in_=bpi[:]), reads=["bpi"], writes=["bif"])
                S.op("dve", lambda e: e.tensor_copy(out=bjf[:], in_=bpj[:]), reads=["bpj"], writes=["bjf"])
            A(posij)

            def kx(pf_, c2, kf):
                S.op("dve", lambda e: e.tensor_tensor(out=eq[:], in0=iota16[:].unsqueeze(1).to_broadcast([P, 128, 16]),
                                                      in1=pf_[:].rearrange("p h r -> p (h r)").unsqueeze(2).to_broadcast([P, 128, 16]),
                                                      op=ALU.is_equal),
                     reads=["iota16", "bif", "bjf"], writes=["eq"])
                S.op("dve", lambda e: e.tensor_tensor(out=eq[:].rearrange("p (h r) i -> p h r i", h=8),
                                                      in0=eq[:].rearrange("p (h r) i -> p h r i", h=8),
                                                      in1=tif[:, c2, :, :].unsqueeze(2).to_broadcast([P, 8, 16, 16]), op=ALU.mult),
                     reads=["eq", "tif"], writes=["eq"])
                S.op("dve", lambda e: e.tensor_reduce(out=kf[:], in_=eq[:], axis=AX.X, op=ALU.add), reads=["eq"], writes=["kf%d" % c2])
            A(lambda: kx(bif, 0, k1f))
            A(lambda: kx(bjf, 1, k2f))

            def fin():
                S.op("dve", lambda e: e.scalar_tensor_tensor(out=k1f[:], in0=k1f[:], scalar=128.0, in1=k2f[:], op0=ALU.mult, op1=ALU.add),
                     reads=["kf0", "kf1"], writes=["kf0"])
                S.op("dve", lambda e: e.tensor_copy(out=eidx[b][:], in_=k1f[:]), reads=["kf0"], writes=["eidx%d" % b])
                S.op("dve", lambda e: e.tensor_tensor(out=gate[b][:], in0=bs[:], in1=bs[:, :, 0:1].to_broadcast([P, 8, 16]), op=ALU.subtract),
                     reads=bsk, writes=["gate%d" % b])
                S.op("act", lambda e: e.activation(out=gate[b][:], in_=gate[b][:], func=AF.Exp), reads=["gate%d" % b], writes=["gate%d" % b])
                S.op("dve", lambda e: e.tensor_reduce(out=gsum[:], in_=gate[b][:], axis=AX.X, op=ALU.add), reads=["gate%d" % b], writes=["gsum"])
                S.op("dve", lambda e: e.reciprocal(out=gsum[:], in_=gsum[:]), reads=["gsum"], writes=["gsum"])
                S.op("dve", lambda e: e.tensor_tensor(out=gate[b][:], in0=gate[b][:], in1=gsum[:].unsqueeze(2).to_broadcast([P, 8, 16]), op=ALU.mult),
                     reads=["gate%d" % b, "gsum"], writes=["gate%d" % b])
            A(fin)
            return st

        gcount = [0]

        def slot(t, sl):
            b = t % 2
            gb = gcount[0] % NG
            gcount[0] += 1
            pb_ = sl % NPR
            db = sl % 4
            S.dma("pool", lambda e: e.indirect_dma_start(out=uv[gb][:], out_offset=None, in_=puv2,
                                                         in_offset=bass.IndirectOffsetOnAxis(ap=eidx[b][:, sl:sl + 1], axis=0)),
                  "uv%d" % gb, reads=["eidx%d" % b], writes=["uv%d" % gb])
            S.op("dve", lambda e: e.tensor_tensor(out=prod[pb_][:], in0=uv[gb][:, 0:D], in1=h3b[b][:], op=ALU.mult),
                 reads=["uv%d" % gb, "h3b%d" % b], writes=["prod%d" % pb_])
            ag = (sl // 4) % 4
            if sl % DVEACC == DVEACC - 1:
                S.op("dve", lambda e: e.tensor_scalar(out=junk3[:], in0=prod[pb_][:], scalar1=1.0, scalar2=None, op0=ALU.mult, op1=ALU.add,
                                                      accum_out=aval[:, sl:sl + 1]),
                     reads=["prod%d" % pb_], writes=["aval%d_%d" % (ag, sl % 4)])
            else:
                S.op("act", lambda e: e.activation(out=junk2[:], in_=prod[pb_][:], func=AF.Copy, accum_out=aval[:, sl:sl + 1]),
                     reads=["prod%d" % pb_], writes=["aval%d_%d" % (ag, sl % 4)])
            if sl % 4 == 3:
                S.op("act", lambda e: e.activation(out=gl[:, sl - 3:sl + 1], in_=aval[:, sl - 3:sl + 1], func=AF.Gelu),
                     reads=["aval%d_%d" % (ag, i) for i in range(4)], writes=["gl%d" % ag])
            pend.append((t, sl, gb))
            if len(pend) > LAG:
                second(*pend.pop(0))

        LAG = 6
        pend = []

        def second(t, sl, gb):
            b = t % 2
            db = sl % 4
            ag = (sl // 4) % 4
            gfl = gate[b][:].rearrange("p h r -> p (h r)")
            S.op("dve", lambda e: e.tensor_scalar(out=dg[db][:], in0=ident[:], scalar1=gl[:, sl:sl + 1], scalar2=gfl[:, sl:sl + 1],
                                                  op0=ALU.mult, op1=ALU.mult),
                 reads=["ident", "gl%d" % ag, "gate%d" % b], writes=["dg%d" % db])
            ab = 4 + 2 * b
            for hf in range(2):
                S.op("pe", lambda e: e.matmul(PS[:, ab + hf, :], lhsT=dg[db][:], rhs=uv[gb][:, D + hf * 512:D + (hf + 1) * 512],
                                              start=(sl == 0), stop=(sl == 127)),
                     reads=["dg%d" % db, "uv%d" % gb], writes=pk(ab + hf))
            if sl == 127:
                finalize(t)

        def finalize(t):
            b = t % 2
            ab = 4 + 2 * b
            S.op("dve", lambda e: e.tensor_tensor(out=x3[:], in0=PS[:, ab:ab + 2, :].rearrange("p a b -> p (a b)"), in1=x2[b][:], op=ALU.add),
                 reads=pk(ab, ab + 1) + ["x2%d" % b], writes=["x3"])
            rmsnorm_tile(x3[:], "x3", gfin, ot[b][:], "ot%d" % b, "P2", junk2, ssq2, rstd2, gkey="gainf")
            S.dma("sp", lambda e: e.dma_start(out=outT[t], in_=ot[b][:]), "out", reads=["ot%d" % b])

        ntc = NT
        for f_ in topk_steps(0):
            f_()
        for t in range(ntc):
            nxt = topk_steps(t + 1) if t + 1 < ntc else []
            ni = 0
            for sl in range(128):
                slot(t, sl)
                if sl >= 16 and ni < len(nxt):
                    want = ((sl - 15) * len(nxt) + 103) // 104
                    while ni < min(want, len(nxt)):
                        nxt[ni]()
                        ni += 1
            while ni < len(nxt):
                nxt[ni]()
                ni += 1
        while pend:
            second(*pend.pop(0))
        S.barrier()

    S.barrier()
    gstack.close()
    return nc, S


_CACHE = {}


def kernel(x, mem, positions, norm_mix_g, w_in, da_lambda, da_subln_g, w_branch_a, w_branch_b, gate_bias, w_out,
           norm_mem_g, mem_kv_norm_g, w_mem_q, w_mem_kv, w_mem_o, norm_ffn_g, peer_w_q, peer_sub_keys, peer_u, peer_v,
           final_norm_g):
    n = 8
    f = lambda a: np.ascontiguousarray(np.asarray(a))
    if "nc" not in _CACHE:
        _CACHE["nc"] = build_program()[0]
    nc = _CACHE["nc"]
    skt = f(np.asarray(peer_sub_keys)[0].transpose(1, 3, 0, 2).reshape(128, 8, 128))
    shared = {
        "norm_mix_g": f(norm_mix_g[0]), "w_in": f(w_in[0]), "da_lambda": f(np.asarray(da_lambda)[0].reshape(256)),
        "da_subln_g": f(da_subln_g[0]), "w_branch_a": f(w_branch_a[0]), "w_branch_b": f(w_branch_b[0]),
        "gate_bias": f(gate_bias[0]), "w_out": f(w_out[0]), "norm_mem_g": f(norm_mem_g[0]),
        "mem_kv_norm_g": f(mem_kv_norm_g[0]), "w_mem_q": f(w_mem_q[0]), "w_mem_kv": f(w_mem_kv[0]),
        "w_mem_o": f(w_mem_o[0]), "norm_ffn_g": f(norm_ffn_g[0]), "peer_w_q": f(peer_w_q[0]), "peer_skt": skt,
        "peer_u": f(peer_u[0]), "peer_v": f(peer_v[0]), "final_norm_g": f(final_norm_g),
    }
    in_maps = []
    for b in range(n):
        m = dict(shared)
        m["x"] = f(x[b])
        m["mem"] = f(mem[b])
        m["pos"] = f(np.asarray(positions)[b].astype(np.int32).reshape(NT, P).T)
        in_maps.append(m)
    res = run_bass_kernel_spmd(nc, in_maps, core_ids=list(range(n)))
    return np.stack([np.asarray(r["out"]) for r in res.results], axis=0).astype(np.float32)
```
